# Optimizing a Trainium2 kernel written in Bass

```python
import math
import jax, jax.numpy as jnp
from jax import lax
import numpy as np

D_MODEL = 2048
BATCH = 4
SEQ = 8192
DEPTH = 1

D_PLE = 256
EPS = 1e-6
POOL_WINDOWS = (2, 4, 8, 16)
N_POOL_GROUPS = 4
D_POOL = D_MODEL // 2
POOL_GW = D_POOL // N_POOL_GROUPS
DN_HEADS = 8
DN_DK = 128
DN_DV = 128
D_DN_QK = DN_HEADS * DN_DK
D_DN_V = DN_HEADS * DN_DV
D_QKV = 2 * D_DN_QK + D_DN_V
CONV_K = 4
CHUNK = 64
D_IN = D_POOL + D_QKV + D_DN_V + 2 * DN_HEADS + 2 * D_MODEL
N_GROUPS = 4
EXPERTS_PER_GROUP = 8
N_EXPERTS = N_GROUPS * EXPERTS_PER_GROUP
TOP_K = 2
D_EXPERT = 512
MOE_BLOCK = 128

kernel_name = "hybrid_pool_deltanet_hmoe_block"


def rms_norm(x, gain):
    xf = x.astype(jnp.float32)
    y = xf * lax.rsqrt(jnp.mean(xf * xf, axis=-1, keepdims=True) + EPS)
    return (y * gain.astype(jnp.float32)).astype(x.dtype)


def l2_normalize(x):
    return x * lax.rsqrt(jnp.sum(x * x, axis=-1, keepdims=True) + EPS)


def split_columns(proj):
    sizes = (D_POOL, D_QKV, D_DN_V, DN_HEADS, DN_HEADS, D_MODEL, D_MODEL)
    parts = []
    start = 0
    for size in sizes:
        parts.append(proj[..., start:start + size])
        start += size
    return parts


def causal_pool_mixer(u, w_grp, scale):
    bsz, s, _ = u.shape
    ug = u.astype(jnp.float32).reshape(bsz, s, N_POOL_GROUPS, POOL_GW)
    t1 = jnp.arange(1, s + 1, dtype=jnp.float32)
    outs = []
    for gi, w in enumerate(POOL_WINDOWS):
        xg = ug[:, :, gi]
        c = jnp.cumsum(xg, axis=1)
        cp = jnp.pad(c, ((0, 0), (w, 0), (0, 0)))
        win = cp[:, w:] - cp[:, :s]
        cnt = jnp.minimum(t1, float(w))
        outs.append(win / cnt[None, :, None] - xg)
    d = jnp.stack(outs, axis=2).astype(u.dtype)
    y = jnp.einsum('bsgc,gcd->bsgd', d, w_grp).reshape(bsz, s, D_POOL)
    return y * scale


def causal_short_conv(u, w):
    s = u.shape[1]
    up = jnp.pad(u, ((0, 0), (CONV_K - 1, 0), (0, 0)))
    y = up[:, 0:s] * w[0]
    for k in range(1, CONV_K):
        y = y + up[:, k:k + s] * w[k]
    return jax.nn.silu(y)


def gated_delta_rule(q, k, v, g, beta):
    bsz, s, h, dk = q.shape
    dv = v.shape[-1]
    nc = s // CHUNK
    f32 = jnp.float32
    q = l2_normalize(q.astype(f32)) * (dk ** -0.5)
    k = l2_normalize(k.astype(f32))
    v = v.astype(f32)

    def to_chunks(t):
        return t.reshape(bsz, nc, CHUNK, h, t.shape[-1]).transpose(1, 0, 3, 2, 4)

    qc, kc, vc = to_chunks(q), to_chunks(k), to_chunks(v)
    gc = to_chunks(g.astype(f32)[..., None])[..., 0]
    bc = to_chunks(beta.astype(f32)[..., None])[..., 0]
    gcum = jnp.cumsum(gc, axis=-1)
    incl = jnp.tril(jnp.ones((CHUNK, CHUNK), dtype=bool))
    strict = jnp.tril(jnp.ones((CHUNK, CHUNK), dtype=bool), -1)
    decay = jnp.exp(jnp.where(incl, gcum[..., :, None] - gcum[..., None, :], -jnp.inf))
    kb = kc * bc[..., None]
    lower = jnp.where(strict, jnp.einsum('nbhid,nbhjd->nbhij', kb, kc) * decay, 0.0)
    a_mat = lower + jnp.eye(CHUNK, dtype=f32)
    rhs = jnp.concatenate([vc * bc[..., None], kb * jnp.exp(gcum)[..., None]], axis=-1)
    sol = lax.linalg.triangular_solve(a_mat, rhs, left_side=True, lower=True, unit_diagonal=True)
    u_c, w_c = sol[..., :dv], sol[..., dv:]

    def step(state, inp):
        q_i, k_i, u_i, w_i, g_i, dec_i = inp
        attn = jnp.einsum('bhid,bhjd->bhij', q_i, k_i) * dec_i
        v_new = u_i - jnp.einsum('bhcd,bhde->bhce', w_i, state)
        o = (jnp.einsum('bhcd,bhde->bhce', q_i * jnp.exp(g_i)[..., None], state)
             + jnp.einsum('bhij,bhje->bhie', attn, v_new))
        g_last = g_i[..., -1]
        k_dec = k_i * jnp.exp(g_last[..., None] - g_i)[..., None]
        state = state * jnp.exp(g_last)[..., None, None] + jnp.einsum('bhcd,bhce->bhde', k_dec, v_new)
        return state, o

    state0 = jnp.zeros((bsz, h, dk, dv), dtype=f32)
    _, o = lax.scan(step, state0, (qc, kc, u_c, w_c, gcum, decay))
    return o.transpose(1, 0, 3, 2, 4).reshape(bsz, s, h, dv)


def hierarchical_moe(xt, w_rg, b_rg, w_re, b_re, w_gate, w_up, w_down):
    t = xt.shape[0]
    xf = xt.astype(jnp.float32)
    logits_grp = xf @ w_rg.astype(jnp.float32) + b_rg.astype(jnp.float32)
    p_grp = jax.nn.softmax(logits_grp, axis=-1)
    gi = jnp.argmax(logits_grp, axis=-1)
    p_sel = jnp.take_along_axis(p_grp, gi[:, None], axis=1)[:, 0]
    logits_exp = (xf @ w_re.astype(jnp.float32) + b_re.astype(jnp.float32)).reshape(t, N_GROUPS, EXPERTS_PER_GROUP)
    le = jnp.take_along_axis(logits_exp, gi[:, None, None], axis=1)[:, 0]
    top_vals, top_idx = lax.top_k(le, TOP_K)
    weights = p_sel[:, None] * jax.nn.softmax(top_vals, axis=-1)
    expert_ids = gi[:, None] * EXPERTS_PER_GROUP + top_idx

    n_assign = t * TOP_K
    flat_e = expert_ids.reshape(n_assign).astype(jnp.int32)
    flat_w = weights.reshape(n_assign)
    order = jnp.argsort(flat_e)
    sorted_e = flat_e[order]
    sorted_tok = (order // TOP_K).astype(jnp.int32)
    sorted_w = flat_w[order]
    counts = jnp.bincount(flat_e, length=N_EXPERTS)
    starts = jnp.cumsum(counts) - counts
    padded = ((counts + MOE_BLOCK - 1) // MOE_BLOCK) * MOE_BLOCK
    pends = jnp.cumsum(padded)
    pstarts = pends - padded
    pos = pstarts[sorted_e] + (jnp.arange(n_assign) - starts[sorted_e])
    cap = n_assign + N_EXPERTS * MOE_BLOCK
    n_blocks = cap // MOE_BLOCK
    tok_pad = jnp.zeros((cap,), jnp.int32).at[pos].set(sorted_tok)
    w_pad = jnp.zeros((cap,), xt.dtype).at[pos].set(sorted_w.astype(xt.dtype))
    blk_e = jnp.minimum(jnp.searchsorted(pends, jnp.arange(n_blocks) * MOE_BLOCK, side='right'), N_EXPERTS - 1)

    def blk_step(acc, inp):
        idx, wt, e = inp
        xb = xt[idx]
        hmid = jax.nn.silu(xb @ w_gate[e]) * (xb @ w_up[e])
        yb = hmid @ w_down[e]
        return acc.at[idx].add(yb * wt[:, None]), None

    acc, _ = lax.scan(blk_step, jnp.zeros_like(xt),
                      (tok_pad.reshape(n_blocks, MOE_BLOCK), w_pad.reshape(n_blocks, MOE_BLOCK), blk_e))
    return acc


def setup_inputs(seed: int = 0) -> dict:
    key = jax.random.key(seed)
    ks = jax.random.split(key, 32)
    nrm = jax.random.normal
    f32 = jnp.float32
    x = nrm(ks[0], (BATCH, SEQ, D_MODEL), f32)
    p = nrm(ks[1], (DEPTH, BATCH, SEQ, D_PLE), f32)
    norm_mix = 1.0 + 0.02 * nrm(ks[2], (DEPTH, D_MODEL), f32)
    w_in = nrm(ks[3], (DEPTH, D_MODEL, D_IN), f32) * D_MODEL ** -0.5
    pool_w = nrm(ks[4], (DEPTH, N_POOL_GROUPS, POOL_GW, POOL_GW), f32) * POOL_GW ** -0.5
    pool_scale = 1.0 + 0.1 * nrm(ks[5], (DEPTH, D_POOL), f32)
    conv_w = nrm(ks[6], (DEPTH, CONV_K, D_QKV), f32) * CONV_K ** -0.5
    a_log = jnp.log(jax.random.uniform(ks[7], (DEPTH, DN_HEADS), f32, minval=1.0, maxval=16.0))
    dt = jnp.exp(jax.random.uniform(ks[8], (DEPTH, DN_HEADS), f32, minval=math.log(1e-3), maxval=math.log(0.1)))
    dt_bias = dt + jnp.log(-jnp.expm1(-dt))
    dn_norm = 1.0 + 0.02 * nrm(ks[9], (DEPTH, DN_DV), f32)
    w_up_pool = nrm(ks[10], (DEPTH, D_POOL, D_MODEL), f32) * D_POOL ** -0.5
    w_up_dn = nrm(ks[11], (DEPTH, D_DN_V, D_MODEL), f32) * D_DN_V ** -0.5
    w_out = nrm(ks[12], (DEPTH, D_MODEL, D_MODEL), f32) * D_MODEL ** -0.5
    norm_moe = 1.0 + 0.02 * nrm(ks[13], (DEPTH, D_MODEL), f32)
    w_router_group = nrm(ks[14], (DEPTH, D_MODEL, N_GROUPS), f32) * D_MODEL ** -0.5
    b_router_group = 0.01 * nrm(ks[15], (DEPTH, N_GROUPS), f32)
    w_router_expert = nrm(ks[16], (DEPTH, D_MODEL, N_EXPERTS), f32) * D_MODEL ** -0.5
    b_router_expert = 0.01 * nrm(ks[17], (DEPTH, N_EXPERTS), f32)
    w_gate = nrm(ks[18], (DEPTH, N_EXPERTS, D_MODEL, D_EXPERT), f32) * D_MODEL ** -0.5
    w_up = nrm(ks[19], (DEPTH, N_EXPERTS, D_MODEL, D_EXPERT), f32) * D_MODEL ** -0.5
    w_down = nrm(ks[20], (DEPTH, N_EXPERTS, D_EXPERT, D_MODEL), f32) * D_EXPERT ** -0.5
    norm_ple = 1.0 + 0.02 * nrm(ks[21], (DEPTH, D_MODEL), f32)
    w_ple_gate = nrm(ks[22], (DEPTH, D_MODEL, D_MODEL), f32) * D_MODEL ** -0.5
    w_ple_proj = nrm(ks[23], (DEPTH, D_PLE, D_MODEL), f32) * D_PLE ** -0.5
    norm_final = 1.0 + 0.02 * nrm(ks[24], (D_MODEL,), f32)
    return {"x": x, "p": p, "norm_mix": norm_mix, "w_in": w_in, "pool_w": pool_w,
            "pool_scale": pool_scale, "conv_w": conv_w, "a_log": a_log, "dt_bias": dt_bias,
            "dn_norm": dn_norm, "w_up_pool": w_up_pool, "w_up_dn": w_up_dn, "w_out": w_out,
            "norm_moe": norm_moe, "w_router_group": w_router_group, "b_router_group": b_router_group,
            "w_router_expert": w_router_expert, "b_router_expert": b_router_expert,
            "w_gate": w_gate, "w_up": w_up, "w_down": w_down, "norm_ple": norm_ple,
            "w_ple_gate": w_ple_gate, "w_ple_proj": w_ple_proj, "norm_final": norm_final}


def reference(x, p, norm_mix, w_in, pool_w, pool_scale, conv_w, a_log, dt_bias, dn_norm,
              w_up_pool, w_up_dn, w_out, norm_moe, w_router_group, b_router_group,
              w_router_expert, b_router_expert, w_gate, w_up, w_down, norm_ple,
              w_ple_gate, w_ple_proj, norm_final):
    bsz, s, d = x.shape
    h = x
    for i in range(DEPTH):
        n1 = rms_norm(h, norm_mix[i])
        proj = n1 @ w_in[i]
        u_pool, qkv, z, b_lin, a_lin, gate_pool, gate_dn = split_columns(proj)
        y_pool = causal_pool_mixer(u_pool, pool_w[i], pool_scale[i])
        qkv = causal_short_conv(qkv, conv_w[i])
        q = qkv[..., :D_DN_QK].reshape(bsz, s, DN_HEADS, DN_DK)
        k = qkv[..., D_DN_QK:2 * D_DN_QK].reshape(bsz, s, DN_HEADS, DN_DK)
        v = qkv[..., 2 * D_DN_QK:].reshape(bsz, s, DN_HEADS, DN_DV)
        beta = jax.nn.sigmoid(b_lin.astype(jnp.float32))
        g = -jnp.exp(a_log[i].astype(jnp.float32)) * jax.nn.softplus(a_lin.astype(jnp.float32) + dt_bias[i].astype(jnp.float32))
        o = gated_delta_rule(q, k, v, g, beta)
        o = rms_norm(o, dn_norm[i]) * jax.nn.silu(z.astype(jnp.float32).reshape(bsz, s, DN_HEADS, DN_DV))
        y_dn = o.reshape(bsz, s, D_DN_V).astype(x.dtype)
        merged = (jax.nn.sigmoid(gate_pool) * (y_pool @ w_up_pool[i])
                  + jax.nn.sigmoid(gate_dn) * (y_dn @ w_up_dn[i]))
        h = h + merged @ w_out[i]
        n2 = rms_norm(h, norm_moe[i])
        y_moe = hierarchical_moe(n2.reshape(bsz * s, d), w_router_group[i], b_router_group[i],
                                 w_router_expert[i], b_router_expert[i], w_gate[i], w_up[i], w_down[i])
        h = h + y_moe.reshape(bsz, s, d)
        n3 = rms_norm(h, norm_ple[i])
        h = h + jax.nn.sigmoid(n3 @ w_ple_gate[i]) * (p[i] @ w_ple_proj[i])
    return rms_norm(h, norm_final)
```

```python
import numpy as np
import concourse.bass as bass
import concourse.mybir as mybir
from concourse.bass_utils import run_bass_kernel_spmd
from contextlib import ExitStack

F32 = mybir.dt.float32
BF16 = mybir.dt.bfloat16
I32 = mybir.dt.int32
AF = mybir.ActivationFunctionType
ALU = mybir.AluOpType
AX = mybir.AxisListType
SEM_MAX = 1800
NEG = -30000.0
EPS = 1e-6


class Cfg:
    def __init__(self, **kw):
        self.D = 2048; self.DP = 1024; self.H = 8; self.NGR = 4; self.EPG = 8; self.DE = 512
        self.DPLE = 256; self.TOK = 4096; self.T = 256; self.CAP = 384; self.WINS = (2, 4, 8, 16)
        self.__dict__.update(kw)
        self.GW = self.DP // 4
        self.DQK = self.H * 128
        self.DIN = self.DP + 4 * self.DQK + 2 * self.H + 2 * self.D
        self.NE = self.NGR * self.EPG
        self.KD = self.D // 128
        self.HG = min(4, self.H)
        self.o_pool = 0
        self.o_q = self.DP
        self.o_k = self.DP + self.DQK
        self.o_v = self.DP + 2 * self.DQK
        self.o_z = self.DP + 3 * self.DQK
        self.o_b = self.DP + 4 * self.DQK
        self.o_a = self.o_b + self.H
        self.o_gp = self.o_a + self.H
        self.o_gd = self.o_gp + self.D


class Tile:
    def __init__(self, name, ap, root=None):
        self.name = name
        self.ap = ap
        self.root = root.root if root is not None else self
        if root is None:
            self._last_w = None
            self._readers = []

    @property
    def last_w(self):
        return self.root._last_w

    @last_w.setter
    def last_w(self, v):
        self.root._last_w = v

    @property
    def readers(self):
        return self.root._readers

    @readers.setter
    def readers(self, v):
        self.root._readers = v

    def __getitem__(self, k):
        return self.ap[k]


class Prog:
    ENGS = ("pe", "act", "dve", "pool", "sp")

    def __init__(self, nc, es):
        self.nc = nc
        self.es = es
        self.lists = {e: [] for e in self.ENGS}
        self.cnt = {e: 0 for e in self.ENGS}
        self.esems = {e: [] for e in self.ENGS}
        self.waited = {e: {} for e in self.ENGS}
        self.nsem = 0
        self.dq = {"sp": [], "pool": [], "act": []}
        self.dq_i = {"sp": 0, "pool": 0, "act": 0}
        self.NDQ = 12
        self.all_dma_tokens = {}

    def new_sem(self, name):
        self.nsem += 1
        return self.es.enter_context(self.nc.semaphore(f"{name}_{self.nsem}"))

    def eng_token(self, eng):
        n = self.cnt[eng]
        ep, v = divmod(n, SEM_MAX)
        while len(self.esems[eng]) <= ep:
            self.esems[eng].append(self.new_sem(f"e_{eng}"))
        self.cnt[eng] = n + 1
        return ("e", eng, ep, v + 1)

    def _need(self, eng, toks):
        out = []
        w = self.waited[eng]
        for t in toks:
            if t is None:
                continue
            if t[0] == "e":
                _, pe, ep, v = t
                if pe == eng and eng == "pe":
                    continue
                key = ("e", pe)
                cur = w.get(key, (-1, 0))
                if (ep, v) <= cur:
                    continue
                w[key] = (ep, v)
                out.append((self.esems[pe][ep], v))
            else:
                _, sem, v, sid = t
                cur = w.get(sid, 0)
                if v <= cur:
                    continue
                w[sid] = v
                out.append((sem, v))
        return out

    def _deps(self, reads, writes):
        toks = []
        for t in reads:
            toks.append(t.last_w)
        for t in writes:
            toks.append(t.last_w)
            toks.extend(t.readers)
        return toks

    def op(self, eng, emit, reads=(), writes=()):
        waits = self._need(eng, self._deps(reads, writes))
        tok = self.eng_token(eng)
        sem = self.esems[eng][tok[2]]
        L = self.lists[eng]

        def run(e, waits=waits, emit=emit, sem=sem):
            for s, v in waits:
                e.wait_ge(s, v)
            emit(e).then_inc(sem, 1)
        L.append(run)
        for t in reads:
            t.readers.append(tok)
        for t in writes:
            t.last_w = tok
            t.readers = []
        return tok

    def dma(self, q, emit, reads=(), writes=()):
        lst = self.dq[q]
        i = self.dq_i[q]
        self.dq_i[q] = (i + 1) % self.NDQ
        if len(lst) <= i:
            lst.append([self.new_sem(f"d_{q}"), 0, None])
        ent = lst[i]
        if ent[1] + 16 > SEM_MAX:
            ent[0] = self.new_sem(f"d_{q}")
            ent[1] = 0
        prev_tok = ent[2]
        ent[1] += 16
        sem, val = ent[0], ent[1]
        tok = ("d", sem, val, id(sem))
        ent[2] = tok
        waits = self._need(q, self._deps(reads, writes) + [prev_tok])
        L = self.lists[q]

        def run(e, waits=waits, emit=emit, sem=sem):
            for s, v in waits:
                e.wait_ge(s, v)
            emit(e).then_inc(sem, 16)
        L.append(run)
        for t in reads:
            t.readers.append(tok)
        for t in writes:
            t.last_w = tok
            t.readers = []
        self.all_dma_tokens[id(sem)] = tok
        return tok

    def wait_tokens(self, eng, toks):
        waits = self._need(eng, toks)
        if not waits:
            return

        def run(e, waits=waits):
            for s, v in waits:
                e.wait_ge(s, v)
        self.lists[eng].append(run)

    def barrier(self):
        toks = list(self.all_dma_tokens.values())
        for e in self.ENGS:
            n = self.cnt[e]
            if n > 0:
                ep, v = divmod(n - 1, SEM_MAX)
                toks.append(("e", e, ep, v + 1))
        for e in self.ENGS:
            self.wait_tokens(e, toks)

    def finalize(self):
        final = list(self.all_dma_tokens.values())
        self.wait_tokens("sp", final)
        self.wait_tokens("pool", final)
        block = self.es.enter_context(self.nc.Block())
        lists = self.lists

        @block.tensor
        def _(e):
            for f in lists["pe"]:
                f(e)

        @block.scalar
        def _(e):
            for f in lists["act"]:
                f(e)

        @block.vector
        def _(e):
            for f in lists["dve"]:
                f(e)

        @block.gpsimd
        def _(e):
            for f in lists["pool"]:
                f(e)

        @block.sync
        def _(e):
            for f in lists["sp"]:
                f(e)


def bc(ap, shape):
    return ap.to_broadcast(list(shape))


def build(cfg, prefix=True):
    c = cfg
    D, DP, H, KD, T, TOK = c.D, c.DP, c.H, c.KD, c.T, c.TOK
    NSUB = T // 128
    NBLK = TOK // T
    HG = c.HG
    NHG = H // HG
    DQK = c.DQK
    NE, CAP = c.NE, c.CAP
    NCAPT = CAP // 128
    nc = bass.Bass("TRN2", target_bir_lowering=False)
    es = ExitStack()
    pg = Prog(nc, es)

    def din(name, shape, dt=F32):
        return nc.dram_tensor(name, list(shape), dt, kind="ExternalInput").ap()

    x_d = din("x", [TOK, D])
    xp_d = din("xpre", [TOK, D])
    xw_d = din("xwarm", [128, D])
    p_d = din("p", [TOK, c.DPLE])
    norm_mix_d = din("norm_mix", [D]); w_in_d = din("w_in", [D, c.DIN])
    pool_w_d = din("pool_w", [4, c.GW, c.GW]); pool_scale_d = din("pool_scale", [DP])
    conv_w_d = din("conv_w", [4, 3 * DQK]); a_log_d = din("a_log", [H]); dt_bias_d = din("dt_bias", [H])
    dn_norm_d = din("dn_norm", [128]); w_up_pool_d = din("w_up_pool", [DP, D]); w_up_dn_d = din("w_up_dn", [DQK, D])
    w_out_d = din("w_out", [D, D]); norm_moe_d = din("norm_moe", [D])
    w_rg_d = din("w_router_group", [D, c.NGR]); b_rg_d = din("b_router_group", [c.NGR])
    w_re_d = din("w_router_expert", [D, NE]); b_re_d = din("b_router_expert", [NE])
    w_gate_d = din("w_gate", [NE, D, c.DE]); w_upe_d = din("w_up", [NE, D, c.DE]); w_down_d = din("w_down", [NE, c.DE, D])
    norm_ple_d = din("norm_ple", [D]); w_pg_d = din("w_ple_gate", [D, D]); w_pp_d = din("w_ple_proj", [c.DPLE, D])
    norm_final_d = din("norm_final", [D])
    cst_d = din("cst", [128, 1024])
    band_d = din("band", [4, 3, 128, 128])
    out_d = nc.dram_tensor("out", [TOK, D], F32, kind="ExternalOutput").ap()
    h1s_d = nc.dram_tensor("h1s", [TOK, D], F32, kind="Internal").ap()
    xe_d = nc.dram_tensor("xe", [NE * CAP, D], BF16, kind="Internal").ap()
    ye_d = nc.dram_tensor("ye", [NE * CAP, D], F32, kind="Internal").ap()

    ARENA_COLS = 51 * 1024 + 512
    arena = es.enter_context(nc.sbuf_tensor("arena", [128, ARENA_COLS], F32))
    aoff = [0]
    amax = [0]

    def carve(off, shape, dt):
        free = 1
        for d in shape[1:]:
            free *= d
        ncol = free if dt == F32 or dt == I32 else (free + 1) // 2
        ap = arena[0:shape[0], off:off + ncol]
        if dt != F32:
            ap = ap.bitcast(dt)[:, 0:free]
        if len(shape) == 3:
            ap = ap.rearrange("p (a b) -> p a b", a=shape[1])
        elif len(shape) == 4:
            ap = ap.rearrange("p (a b c) -> p a b c", a=shape[1], b=shape[2])
        return ap, ncol

    def sb(name, shape, dt=F32):
        ap, ncol = carve(aoff[0], shape, dt)
        aoff[0] += ncol
        amax[0] = max(amax[0], aoff[0])
        assert aoff[0] <= ARENA_COLS, (name, aoff[0])
        return Tile(name, ap)

    def view(parent_tile, parent_off32, name, shape, dt=F32):
        ap, ncol = carve(parent_tile.base_off + parent_off32, shape, dt)
        return Tile(name, ap, root=parent_tile)

    def sb_b(name, shape, dt=F32):
        off = aoff[0]
        t = sb(name, shape, dt)
        t.base_off = off
        return t

    psb = [Tile(f"ps{i}", es.enter_context(nc.psum_tensor(f"ps{i}", [128, 512], F32))) for i in range(8)]
    ps_i = [0]

    def PS():
        t = psb[ps_i[0] % 8]
        ps_i[0] += 1
        return t

    pool_i = {}

    def PSP(key, banks):
        i = pool_i.get(key, 0)
        pool_i[key] = i + 1
        return psb[banks[i % len(banks)]]

    cst = sb("cst", [128, 1024])
    pg.dma("sp", lambda e: e.dma_start(out=cst[:, :], in_=cst_d), writes=[cst])
    ident = cst.ap[:, 0:128]
    U = cst.ap[:, 128:192]
    Bm = cst.ap[:, 192:256]
    NEGU = cst.ap[:, 256:320]
    SUN = cst.ap[:, 320:384]
    I64 = cst.ap[:, 384:448]
    ONES = cst.ap[:, 448:576]
    SLT = cst.ap[:, 576:704]
    IOTA_E = cst.ap[:, 704:704 + 32]
    identb = sb("identb", [128, 128], BF16)
    pg.op("dve", lambda e: e.tensor_copy(identb[:, :], ident), reads=[cst], writes=[identb])
    onesb = sb("onesb", [128, 128], BF16)
    pg.op("dve", lambda e: e.tensor_copy(onesb[:, :], ONES), reads=[cst], writes=[onesb])
    sltb = sb("sltb", [128, 128], BF16)
    pg.op("dve", lambda e: e.tensor_copy(sltb[:, :], SLT), reads=[cst], writes=[sltb])

    def load_col(name, d_ap, n, dt=F32):
        t = sb(name, [128, n // 128], dt)
        pg.dma("sp", lambda e: e.dma_start(out=t[:, :], in_=d_ap.rearrange("(c p) -> p c", p=128),
                                           allow_slow_non_contiguous=True), writes=[t])
        return t

    def load_rep(name, d_ap, n):
        t = sb(name, [128, n])
        pg.dma("sp", lambda e: e.dma_start(out=t[:, :], in_=d_ap.rearrange("(o n) -> o n", o=1).partition_broadcast(128)),
               writes=[t])
        return t

    gmixT = load_col("gmixT", norm_mix_d, D)
    pscaleT = load_col("pscaleT", pool_scale_d, DP)
    convT = sb("convT", [128, 4, 3 * DQK // 128])
    for k in range(4):
        pg.dma("sp", lambda e, k=k: e.dma_start(out=convT[:, k, :], in_=conv_w_d[k].rearrange("(c p) -> p c", p=128),
                                                allow_slow_non_contiguous=True), writes=[convT])
    gmoeT = load_col("gmoeT", norm_moe_d, D)
    gpleT = load_col("gpleT", norm_ple_d, D)
    g_dn = load_rep("g_dn", dn_norm_d, 128)
    brep = sb("brep", [128, c.NGR + NE])
    pg.dma("sp", lambda e: e.dma_start(out=brep[:, 0:c.NGR], in_=b_rg_d.rearrange("(o n) -> o n", o=1).partition_broadcast(128)), writes=[brep])
    pg.dma("sp", lambda e: e.dma_start(out=brep[:, c.NGR:], in_=b_re_d.rearrange("(o n) -> o n", o=1).partition_broadcast(128)), writes=[brep])
    hp = sb("hp", [128, 2 * H])
    pg.dma("sp", lambda e: e.dma_start(out=hp[:, 0:H], in_=a_log_d.rearrange("(o n) -> o n", o=1).partition_broadcast(128)), writes=[hp])
    pg.dma("sp", lambda e: e.dma_start(out=hp[:, H:], in_=dt_bias_d.rearrange("(o n) -> o n", o=1).partition_broadcast(128)), writes=[hp])
    pg.op("act", lambda e: e.activation(hp[:, 0:H], hp[:, 0:H], AF.Exp), reads=[hp], writes=[hp])
    pg.op("dve", lambda e: e.tensor_scalar(hp[:, 0:H], hp[:, 0:H], -1.0, None, op0=ALU.mult), reads=[hp], writes=[hp])
    wr = sb("wr", [128, KD, c.NGR + NE])
    pg.dma("sp", lambda e: e.dma_start(out=wr[:, :, 0:c.NGR], in_=w_rg_d.rearrange("(c p) n -> p c n", p=128), allow_slow_non_contiguous=True), writes=[wr])
    pg.dma("sp", lambda e: e.dma_start(out=wr[:, :, c.NGR:], in_=w_re_d.rearrange("(c p) n -> p c n", p=128), allow_slow_non_contiguous=True), writes=[wr])
    GK = c.GW // 128
    wgrp = sb("wgrp", [128, 4, GK, c.GW], BF16)
    for g in range(4):
        pg.dma("pool", lambda e, g=g: e.dma_start(out=wgrp[:, g, :, :], in_=pool_w_d[g].rearrange("(c p) n -> p c n", p=128)), writes=[wgrp])
    bandb = sb("bandb", [128, 4, 3, 128], BF16)
    pg.dma("pool", lambda e: e.dma_start(out=bandb[:, :, :, :], in_=band_d.rearrange("g k p n -> p g k n")), writes=[bandb])
    wba = sb("wba", [128, KD, 2 * H], BF16)
    pg.dma("pool", lambda e: e.dma_start(out=wba[:, :, :], in_=w_in_d[:, c.o_b:c.o_b + 2 * H].rearrange("(c p) n -> p c n", p=128)), writes=[wba])
    st1 = sb("st1", [128, 8])
    n1T = sb("n1T", [128, KD, T], BF16)
    sg = sb("sg", [128, 512])
    n2b = sb("n2b", [128, D], BF16)
    pg.op("dve", lambda e: e.memset(n2b[:, :], 0.0), writes=[n2b])
    zero_toks = []
    for r in range(NE * CAP // 128):
        zero_toks.append(pg.dma("sp", lambda e, r=r: e.dma_start(out=xe_d[r * 128:(r + 1) * 128, :], in_=n2b[:, :]), reads=[n2b]))

    NSLOT = 3
    WK = max(KD, DQK // 128, DP // 128)
    wslots = [sb(f"wslot{i}", [128, WK * 512], BF16) for i in range(NSLOT)]
    ws_i = [0]

    def load_w_cast(src2d, kchunks, ncols):
        t = wslots[ws_i[0] % NSLOT]
        ws_i[0] += 1
        view_ = t.ap[:, 0:kchunks * ncols].rearrange("p (c n) -> p c n", c=kchunks)
        pg.dma("pool", lambda e: e.dma_start(out=view_, in_=src2d.rearrange("(c p) n -> p c n", p=128)), writes=[t])
        return t, view_

    wscr = {}

    def prep_w(key, src2d, kchunks, ncols):
        if key in wscr:
            return
        d = nc.dram_tensor(f"wsc_{key}", [128, kchunks * ncols], BF16, kind="Internal").ap()
        tl = Tile(f"wsc_{key}", d)
        pg.dma("pool", lambda e: e.dma_start(out=d.rearrange("p (c n) -> p c n", c=kchunks), in_=src2d.rearrange("(c p) n -> p c n", p=128)), writes=[tl])
        wscr[key] = (tl, d)

    def load_w(key, src2d, kchunks, ncols):
        prep_w(key, src2d, kchunks, ncols)
        tl, d = wscr[key]
        t = wslots[ws_i[0] % NSLOT]
        ws_i[0] += 1
        view_ = t.ap[:, 0:kchunks * ncols].rearrange("p (c n) -> p c n", c=kchunks)
        pg.dma("sp", lambda e: e.dma_start(out=t.ap[:, 0:kchunks * ncols], in_=d), reads=[tl], writes=[t])
        return t, view_

    def groups(total):
        return [(g0, min(512, total - g0)) for g0 in range(0, total, 512)]

    for off_, key_ in ((c.o_k, "k"), (c.o_v, "v")):
        for g0, ncol in groups(DQK):
            prep_w(f"in{off_ + g0}", w_in_d[:, off_ + g0:off_ + g0 + ncol], KD, ncol)
    for g0, ncol in groups(DP):
        prep_w(f"in{c.o_pool + g0}", w_in_d[:, c.o_pool + g0:c.o_pool + g0 + ncol], KD, ncol)
    for g0, ncol in groups(DQK):
        prep_w(f"in{c.o_q + g0}", w_in_d[:, c.o_q + g0:c.o_q + g0 + ncol], KD, ncol)
    for g0, ncol in groups(D):
        prep_w(f"upp{g0}", w_up_pool_d[:, g0:g0 + ncol], DP // 128, ncol)
        prep_w(f"in{c.o_gp + g0}", w_in_d[:, c.o_gp + g0:c.o_gp + g0 + ncol], KD, ncol)
    for g0, ncol in groups(DQK):
        prep_w(f"in{c.o_z + g0}", w_in_d[:, c.o_z + g0:c.o_z + g0 + ncol], KD, ncol)
    for g0, ncol in groups(D):
        prep_w(f"upd{g0}", w_up_dn_d[:, g0:g0 + ncol], DQK // 128, ncol)
        prep_w(f"in{c.o_gd + g0}", w_in_d[:, c.o_gd + g0:c.o_gd + g0 + ncol], KD, ncol)
    for g0, ncol in groups(D):
        prep_w(f"out{g0}", w_out_d[:, g0:g0 + ncol], KD, ncol)
    for g0, ncol in groups(D):
        prep_w(f"pg{g0}", w_pg_d[:, g0:g0 + ncol], KD, ncol)

    S = [sb(f"S{g}", [128, HG, 128]) for g in range(NHG)]
    for g in range(NHG):
        pg.op("dve", lambda e, g=g: e.memset(S[g][:, :, :], 0.0), writes=[S[g]])
    NQ = 3 * DQK // 128
    halo = sb("halo", [128, NQ, 3])
    pg.op("dve", lambda e: e.memset(halo[:, :, :], 0.0), writes=[halo])
    u_prev = sb("u_prev", [128, DP], BF16)
    cnt_bc = sb("cnt_bc", [128, NE])
    pg.op("dve", lambda e: e.memset(cnt_bc[:, :], 0.0), writes=[cnt_bc])
    NST = TOK // 128
    slot_i = sb("slot_i", [128, NST, 2], I32)
    wts = sb("wts", [128, NST, 2])

    phase_mark = aoff[0]
    xt = [sb(f"xt{i}", [128, D]) for i in range(2)]
    xs = [n2b] * 2
    mT = sb_b("mT", [128, KD, T], BF16)
    raw = [sb(f"raw{i}", [128, T + 3]) for i in range(2)]
    cacc = [sb(f"cacc{i}", [128, T]) for i in range(2)]
    QKV32 = max(3 * H * T // 2, NSUB * (DP // 2) + (DP * T // 256), D)
    qkv = sb_b("qkv", [128, QKV32])
    qc = view(qkv, 0, "qc", [128, H, T], BF16)
    kc = view(qkv, H * T // 2, "kc", [128, H, T], BF16)
    vc = view(qkv, H * T, "vc", [128, H, T], BF16)
    u_tm = [view(qkv, i * (DP // 2), f"u_tm{i}", [128, DP], BF16) for i in range(NSUB)]
    dT = view(qkv, NSUB * (DP // 2), "dT", [128, DP // 128, T], BF16)
    ypT = sb("ypT", [128, DP // 128, T], BF16)
    sgd = sb("sgd", [128, KD, T], BF16)
    assert NSUB * (DP // 2) + (DP * T // 256) <= QKV32
    NCH = T // 64
    z_tm = [sb(f"z_tm{i}", [64, DQK], BF16) for i in range(NCH)]
    ba = sb("ba", [64, 2 * H])
    beta = sb("beta", [64, H]); gg = sb("gg", [64, H])
    k_tm = sb("k_tm", [64, H, 128]); k_n = sb("k_n", [64, H, 128], BF16); v_tm = sb("v_tm", [64, H, 128], BF16)
    k_nT = sb("k_nT", [128, H, 64], BF16)
    sq = sb("sq", [64, H, 128]); sqT = sb("sqT", [128, H, 64])
    ssk = sb("ssk", [64, H]); rk = sb("rk", [64, H]); rq = sb("rq", [64, H])
    o_tm = sb("o_tm", [64, H, 128])
    ydn = sb("ydn", [64, H, 128], BF16)
    ydnT = sb("ydnT", [128, H, T], BF16)
    class GB:
        pass
    GBs = []
    for g in range(NHG):
        o = GB()
        o.A_all = sb(f"A_all{g}", [64, HG, 64]); o.decT = sb(f"decT{g}", [64, HG, 64]); o.attnT = sb(f"attnT{g}", [64, HG, 64], BF16)
        o.M2 = sb(f"M2{g}", [64, HG, 64], BF16); o.Pm = [sb(f"Pm{g}{i}", [64, HG, 64], BF16) for i in range(2)]
        o.Qm = [sb(f"Qm{g}{i}", [64, HG, 64], BF16) for i in range(2)]
        o.XT = sb(f"XT{g}", [64, HG, 64], BF16)
        o.eg = sb(f"eg{g}", [64, 2 * HG]); o.egt = sb(f"egt{g}", [128, HG])
        o.kg = sb(f"kg{g}", [64, HG, 128], BF16); o.kdec = sb(f"kdec{g}", [64, HG, 128], BF16)
        o.up_sb = sb(f"up_sb{g}", [64, HG, 128]); o.wT = sb(f"wT{g}", [128, HG, 64], BF16)
        o.vnew = sb(f"vnew{g}", [64, HG, 128], BF16); o.dtmp = sb(f"dtmp{g}", [64, HG, 128])
        o.o1 = sb(f"o1{g}", [64, HG, 128]); o.sc1 = sb(f"sc1{g}", [64, HG])
        o.Sb = sb(f"Sb{g}", [128, HG, 128], BF16)
        pg.op("dve", lambda e, o=o: e.memset(o.Sb[:, :, :], 0.0), writes=[o.Sb])
        GBs.append(o)
    n2T = view(mT, 0, "n2T", [128, KD, 128])
    n2 = view(qkv, 0, "n2", [128, D])
    assert D <= QKV32 and KD * 128 <= KD * T // 2
    lg = sb("lg", [128, c.NGR + NE]); rt = sb("rt", [128, 64]); oh = sb("oh", [128, 2, NE]); ohs = sb("ohs", [128, NE], BF16)
    posf = sb("posf", [128, NE]); tmpe = sb("tmpe", [128, NE]); le = sb("le", [128, c.EPG]); ohg = sb("ohg", [128, c.NGR])
    oh1 = sb("oh1", [128, c.EPG]); oh2 = sb("oh2", [128, c.EPG]); slf = sb("slf", [128, 2])

    w_in_T = lambda c0, n: w_in_d[:, c0:c0 + n]

    def rms_rstd(src_tile, src_ap, dst, junk):
        pg.op("act", lambda e: e.activation(junk[:, :], src_ap, AF.Square, accum_out=dst), reads=[src_tile], writes=[junk, st1])
        pg.op("act", lambda e: e.activation(dst, dst, AF.Ln, scale=1.0 / D, bias=EPS), reads=[st1], writes=[st1])
        pg.op("act", lambda e: e.activation(dst, dst, AF.Exp, scale=-0.5), reads=[st1], writes=[st1])

    def make_n1T(xsrc_d, row0, s):
        xb = xt[s % 2]; xsb = xs[s % 2]
        pg.dma("sp", lambda e: e.dma_start(out=xb[:, :], in_=xsrc_d[row0:row0 + 128, :]), writes=[xb])
        rms_rstd(xb, xb[:, :], st1[:, 0:1], xsb)
        pg.op("act", lambda e: e.activation(xsb[:, :], xb[:, :], AF.Copy, scale=st1[:, 0:1]), reads=[xb, st1], writes=[xsb])
        for c0 in range(0, KD, 8):
            nb = min(8, KD - c0)
            ps = PS()
            psv = ps.ap[:, :].bitcast(BF16)

            def tr(e, c0=c0, nb=nb, psv=psv, xsb=xsb):
                for j in range(nb):
                    r = e.transpose(psv[:, j * 128:(j + 1) * 128], xsb[:, (c0 + j) * 128:(c0 + j + 1) * 128], identb[:, :])
                return r
            pg.op("pe", tr, reads=[xsb, identb], writes=[ps])
            pg.op("dve", lambda e, c0=c0, nb=nb, psv=psv, s=s: e.tensor_tensor(
                n1T[:, c0:c0 + nb, s * 128:(s + 1) * 128],
                psv[:, 0:nb * 128].rearrange("p (c t) -> p c t", c=nb),
                bc(gmixT[:, c0:c0 + nb].unsqueeze(2), [128, nb, 128]), op=ALU.mult),
                reads=[ps, gmixT], writes=[n1T])

    def mm_fm(ps, wt, wview, col0, rhs_tile, rhs_fn, kch, n):
        def f(e):
            for cc in range(kch):
                r = e.matmul(ps.ap[:, 0:n], wview[:, cc, col0:col0 + 128], rhs_fn(cc), start=(cc == 0), stop=(cc == kch - 1))
            return r
        pg.op("pe", f, reads=[wt, rhs_tile], writes=[ps])

    def mm_tm(ps, lhs_tile, lhs_fn, wt, wview, kch, ncols):
        def f(e):
            for cc in range(kch):
                r = e.matmul(ps.ap[:, 0:ncols], lhs_fn(cc), wview[:, cc, 0:ncols], start=(cc == 0), stop=(cc == kch - 1))
            return r
        pg.op("pe", f, reads=[wt, lhs_tile], writes=[ps])

    def conv_tile(ti, dst_tile, dst_ap, ps, n):
        rb = raw[ti % 2]; ca = cacc[ti % 2]
        pg.op("act", lambda e: e.activation(rb[:, 3:3 + n], ps.ap[:, 0:n], AF.Copy), reads=[ps], writes=[rb])
        pg.op("dve", lambda e: e.tensor_copy(rb[:, 0:3], halo[:, ti, :]), reads=[halo], writes=[rb])
        pg.op("dve", lambda e: e.tensor_copy(halo[:, ti, :], rb[:, n:n + 3]), reads=[rb], writes=[halo])
        pg.op("dve", lambda e: e.tensor_scalar(ca[:, 0:n], rb[:, 0:n], convT[:, 0, ti:ti + 1], None, op0=ALU.mult), reads=[rb, convT], writes=[ca])
        for k in range(1, 4):
            pg.op("dve", lambda e, k=k: e.scalar_tensor_tensor(ca[:, 0:n], rb[:, k:k + n], convT[:, k, ti:ti + 1], ca[:, 0:n],
                                                              op0=ALU.mult, op1=ALU.add), reads=[rb, convT, ca], writes=[ca])
        pg.op("act", lambda e: e.activation(dst_ap, ca[:, 0:n], AF.Silu), reads=[ca], writes=[dst_tile])

    def proj_qkv(which, n):
        dst = (qc, kc, vc)[which]
        off = (c.o_q, c.o_k, c.o_v)[which]
        for g0 in range(0, DQK, 512):
            ncol = min(512, DQK - g0)
            wt, wv = load_w(f"in{off + g0}", w_in_T(off + g0, ncol), KD, ncol)
            for j in range(ncol // 128):
                ps = PS()
                mm_fm(ps, wt, wv, j * 128, n1T, lambda cc: n1T[:, cc, 0:n], KD, n)
                hh = (g0 + j * 128) // 128
                conv_tile(which * H + hh, dst, dst[:, hh, 0:n], ps, n)

    def psb16(ps):
        return ps.ap[:, :].bitcast(BF16)

    def tm_from_fm(src, dst, tok0):
        for g in range(NHG):
            ps = PS()
            pv = psb16(ps)

            def f(e, g=g, pv=pv):
                for j in range(HG):
                    r = e.transpose(pv[0:64, j * 128:(j + 1) * 128], src[:, g * HG + j, tok0:tok0 + 64], identb[:, :])
                return r
            pg.op("pe", f, reads=[src, identb], writes=[ps])
            pg.op("act", lambda e, g=g, pv=pv: e.activation(dst[:, g * HG:(g + 1) * HG, :], pv[0:64, 0:HG * 128].rearrange("p (h d) -> p h d", h=HG), AF.Copy),
                  reads=[ps], writes=[dst])

    def beta_g(tok0):
        ps = PS()

        def f(e):
            for cc in range(KD):
                r = e.matmul(ps.ap[0:64, 0:2 * H], n1T[:, cc, tok0:tok0 + 64], wba[:, cc, :], start=(cc == 0), stop=(cc == KD - 1))
            return r
        pg.op("pe", f, reads=[n1T, wba], writes=[ps])
        pg.op("act", lambda e: e.activation(beta[:, :], ps.ap[0:64, 0:H], AF.Sigmoid), reads=[ps], writes=[beta])
        pg.op("dve", lambda e: e.tensor_tensor(ba[:, 0:H], ps.ap[0:64, H:2 * H], hp[0:64, H:2 * H], op=ALU.add), reads=[ps, hp], writes=[ba])
        pg.op("act", lambda e: e.activation(ba[:, 0:H], ba[:, 0:H], AF.Exp), reads=[ba], writes=[ba])
        pg.op("act", lambda e: e.activation(ba[:, 0:H], ba[:, 0:H], AF.Ln, bias=1.0), reads=[ba], writes=[ba])
        pg.op("dve", lambda e: e.tensor_tensor(gg[:, :], ba[:, 0:H], hp[0:64, 0:H], op=ALU.mult), reads=[ba, hp], writes=[gg])

    def r3(ap, h):
        return ap.rearrange("p (h d) -> p h d", h=h)

    def chain(g, tok0, state_only):
        B = GBs[g]
        cbanks = [0, 1, 2] if g == 0 else [3, 4, 5]

        def CPS():
            return PSP(('c', g), cbanks)
        hs = slice(g * HG, (g + 1) * HG)
        Sg = S[g]
        A_all, decT, attnT, M2, Pm, Qm, XT = B.A_all, B.decT, B.attnT, B.M2, B.Pm, B.Qm, B.XT
        eg, egt, kg, kdec, up_sb, wT, vnew, dtmp, o1, sc1, Sb = B.eg, B.egt, B.kg, B.kdec, B.up_sb, B.wT, B.vnew, B.dtmp, B.o1, B.sc1, B.Sb
        I64b = identb[0:64, 0:64]
        pg.op("dve", lambda e: e.tensor_tensor(A_all[:, :, :], bc(U[0:64, :].unsqueeze(1), [64, HG, 64]),
                                               bc(gg[:, hs].unsqueeze(2), [64, HG, 64]), op=ALU.mult), reads=[cst, gg], writes=[A_all])
        psD = CPS()

        def f(e):
            e.matmul(psD.ap[0:64, 0:HG * 64], Bm[0:64, :], A_all[:, :, :].rearrange("p h i -> p (h i)"), start=True, stop=False, skip_group_check=True)
            r = None
            for j in range(HG):
                r = e.matmul(psD.ap[0:64, j * 64:(j + 1) * 64], I64[0:64, :], NEGU[0:64, :], start=False, stop=(j == HG - 1), skip_group_check=True)
            return r
        pg.op("pe", f, reads=[A_all, cst], writes=[psD])
        psG = CPS()

        def f(e):
            e.matmul(psG.ap[0:64, 0:HG], U[0:64, :], gg[:, hs], start=True, stop=True)
            e.matmul(psG.ap[0:64, HG:2 * HG], Bm[0:64, :], gg[:, hs], start=True, stop=True)
            return e.matmul(psG.ap[:, 2 * HG:3 * HG], ONES[0:64, :], gg[:, hs], start=True, stop=True)
        pg.op("pe", f, reads=[gg, cst], writes=[psG])
        yield
        pg.op("act", lambda e: e.activation(decT[:, :, :], r3(psD.ap[0:64, 0:HG * 64], HG), AF.Exp), reads=[psD], writes=[decT])
        pg.op("act", lambda e: e.activation(eg[:, :], psG.ap[0:64, 0:2 * HG], AF.Exp), reads=[psG], writes=[eg])
        pg.op("act", lambda e: e.activation(egt[:, :], psG.ap[:, 2 * HG:3 * HG], AF.Exp), reads=[psG], writes=[egt])
        psK = CPS()

        def f(e):
            for j in range(HG):
                h = g * HG + j
                r = e.matmul(psK.ap[0:64, j * 64:(j + 1) * 64], k_nT[:, h, :], k_nT[:, h, :], start=True, stop=True)
            return r
        pg.op("pe", f, reads=[k_nT], writes=[psK])
        if not state_only:
            psQ = CPS()

            def f(e):
                for j in range(HG):
                    h = g * HG + j
                    r = e.matmul(psQ.ap[0:64, j * 64:(j + 1) * 64], k_nT[:, h, :], qc[:, h, tok0:tok0 + 64], start=True, stop=True)
                return r
            pg.op("pe", f, reads=[k_nT, qc], writes=[psQ])
        yield
        pg.op("dve", lambda e: e.tensor_tensor(o1[:, :, 0:64], r3(psK.ap[0:64, 0:HG * 64], HG), decT[:, :, :], op=ALU.mult), reads=[psK, decT], writes=[o1])
        pg.op("dve", lambda e: e.tensor_tensor(o1[:, :, 0:64], o1[:, :, 0:64], bc(SUN[0:64, :].unsqueeze(1), [64, HG, 64]), op=ALU.mult), reads=[o1, cst], writes=[o1])
        pg.op("dve", lambda e: e.tensor_tensor(M2[:, :, :], o1[:, :, 0:64], bc(beta[:, hs].unsqueeze(2), [64, HG, 64]), op=ALU.mult), reads=[o1, beta], writes=[M2])
        if not state_only:
            pg.op("dve", lambda e: e.tensor_tensor(attnT[:, :, :], r3(psQ.ap[0:64, 0:HG * 64], HG), decT[:, :, :], op=ALU.mult), reads=[psQ, decT], writes=[attnT])
        psT = CPS()
        pvT = psb16(psT)

        def f(e):
            for j in range(HG):
                r = e.transpose(pvT[0:64, j * 64:(j + 1) * 64], M2[:, j, :], I64b)
            return r
        pg.op("pe", f, reads=[M2, identb], writes=[psT])
        yield
        pg.op("act", lambda e: e.activation(Qm[0][:, :, :], r3(pvT[0:64, 0:HG * 64], HG), AF.Copy), reads=[psT], writes=[Qm[0]])
        pg.op("dve", lambda e: e.tensor_tensor(XT[:, :, :], M2[:, :, :], bc(I64[0:64, :].unsqueeze(1), [64, HG, 64]), op=ALU.add), reads=[M2, cst], writes=[XT])
        Pc, Qc = M2, Qm[0]
        for n in range(1, 6):
            Pn, Qn = Pm[n % 2], Qm[n % 2]
            last = (n == 5)
            psQ2 = CPS()

            def f(e, psQ2=psQ2, Pc=Pc, Qc=Qc):
                for j in range(HG):
                    r = e.matmul(psQ2.ap[0:64, j * 64:(j + 1) * 64], Pc[:, j, :], Qc[:, j, :], start=True, stop=True)
                return r
            pg.op("pe", f, reads=[Pc, Qc], writes=[psQ2])
            if not last:
                psP = CPS()

                def f(e, psP=psP, Pc=Pc, Qc=Qc):
                    for j in range(HG):
                        r = e.matmul(psP.ap[0:64, j * 64:(j + 1) * 64], Qc[:, j, :], Pc[:, j, :], start=True, stop=True)
                    return r
                pg.op("pe", f, reads=[Pc, Qc], writes=[psP])
            yield
            pg.op("act", lambda e, psQ2=psQ2, Qn=Qn: e.activation(Qn[:, :, :], r3(psQ2.ap[0:64, 0:HG * 64], HG), AF.Copy), reads=[psQ2], writes=[Qn])
            if not last:
                pg.op("act", lambda e, psP=psP, Pn=Pn: e.activation(Pn[:, :, :], r3(psP.ap[0:64, 0:HG * 64], HG), AF.Copy), reads=[psP], writes=[Pn])
            psX = CPS()

            def f(e, psX=psX, Qn=Qn):
                for j in range(HG):
                    r = e.matmul(psX.ap[0:64, j * 64:(j + 1) * 64], Qn[:, j, :], XT[:, j, :], start=True, stop=True)
                return r
            pg.op("pe", f, reads=[Qn, XT], writes=[psX])
            yield
            pg.op("dve", lambda e, psX=psX: e.tensor_tensor(XT[:, :, :], XT[:, :, :], r3(psX.ap[0:64, 0:HG * 64], HG), op=ALU.add), reads=[psX, XT], writes=[XT])
            Pc, Qc = Pn, Qn
        pg.op("dve", lambda e: e.tensor_tensor(kg[:, :, :], k_n[:, hs, :], bc(eg[:, 0:HG].unsqueeze(2), [64, HG, 128]), op=ALU.mult), reads=[k_n, eg], writes=[kg])
        pg.op("dve", lambda e: e.tensor_tensor(kdec[:, :, :], k_n[:, hs, :], bc(eg[:, HG:2 * HG].unsqueeze(2), [64, HG, 128]), op=ALU.mult), reads=[k_n, eg], writes=[kdec])
        psU = CPS()

        def f(e):
            for j in range(HG):
                r = e.matmul(psU.ap[0:64, j * 128:(j + 1) * 128], XT[:, j, :], v_tm[:, g * HG + j, :], start=True, stop=True)
            return r
        pg.op("pe", f, reads=[XT, v_tm], writes=[psU])
        psW = CPS()

        def f(e):
            for j in range(HG):
                r = e.matmul(psW.ap[:, j * 64:(j + 1) * 64], kg[:, j, :], XT[:, j, :], start=True, stop=True)
            return r
        pg.op("pe", f, reads=[XT, kg], writes=[psW])
        yield
        pg.op("act", lambda e: e.activation(up_sb[:, :, :], r3(psU.ap[0:64, 0:HG * 128], HG), AF.Copy), reads=[psU], writes=[up_sb])
        pg.op("act", lambda e: e.activation(wT[:, :, :], r3(psW.ap[:, 0:HG * 64], HG), AF.Copy), reads=[psW], writes=[wT])
        psWS = CPS()

        def f(e):
            for j in range(HG):
                r = e.matmul(psWS.ap[0:64, j * 128:(j + 1) * 128], wT[:, j, :], Sb[:, j, :], start=True, stop=True)
            return r
        pg.op("pe", f, reads=[wT, Sb], writes=[psWS])
        if not state_only:
            psT1 = CPS()

            def f(e):
                for j in range(HG):
                    r = e.matmul(psT1.ap[0:64, j * 128:(j + 1) * 128], qc[:, g * HG + j, tok0:tok0 + 64], Sb[:, j, :], start=True, stop=True)
                return r
            pg.op("pe", f, reads=[qc, Sb], writes=[psT1])
        yield
        pg.op("dve", lambda e: e.tensor_tensor(dtmp[:, :, :], up_sb[:, :, :], r3(psWS.ap[0:64, 0:HG * 128], HG), op=ALU.subtract), reads=[up_sb, psWS], writes=[dtmp])
        pg.op("dve", lambda e: e.tensor_tensor(vnew[:, :, :], dtmp[:, :, :], bc(beta[:, hs].unsqueeze(2), [64, HG, 128]), op=ALU.mult), reads=[dtmp, beta], writes=[vnew])
        if not state_only:
            pg.op("dve", lambda e: e.tensor_tensor(sc1[:, :], eg[:, 0:HG], rq[:, hs], op=ALU.mult), reads=[eg, rq], writes=[sc1])
            pg.op("dve", lambda e: e.tensor_tensor(o1[:, :, :], r3(psT1.ap[0:64, 0:HG * 128], HG), bc(sc1[:, :].unsqueeze(2), [64, HG, 128]), op=ALU.mult),
                  reads=[psT1, sc1], writes=[o1])
        psS = CPS()

        def f(e):
            for j in range(HG):
                r = e.matmul(psS.ap[:, j * 128:(j + 1) * 128], kdec[:, j, :], vnew[:, j, :], start=True, stop=True)
            return r
        pg.op("pe", f, reads=[kdec, vnew], writes=[psS])
        if not state_only:
            psT2 = CPS()

            def f(e):
                for j in range(HG):
                    r = e.matmul(psT2.ap[0:64, j * 128:(j + 1) * 128], attnT[:, j, :], vnew[:, j, :], start=True, stop=True)
                return r
            pg.op("pe", f, reads=[attnT, vnew], writes=[psT2])
        yield
        pg.op("dve", lambda e: e.tensor_tensor(Sg[:, :, :], Sg[:, :, :], bc(egt[:, :].unsqueeze(2), [128, HG, 128]), op=ALU.mult), reads=[Sg, egt], writes=[Sg])
        pg.op("dve", lambda e: e.tensor_tensor(Sg[:, :, :], Sg[:, :, :], r3(psS.ap[:, 0:HG * 128], HG), op=ALU.add), reads=[Sg, psS], writes=[Sg])
        pg.op("act", lambda e: e.activation(Sb[:, :, :], Sg[:, :, :], AF.Copy), reads=[Sg], writes=[Sb])
        if not state_only:
            pg.op("dve", lambda e: e.tensor_tensor(dtmp[:, :, :], r3(psT2.ap[0:64, 0:HG * 128], HG), bc(rq[:, hs].unsqueeze(2), [64, HG, 128]), op=ALU.mult),
                  reads=[psT2, rq], writes=[dtmp])
            pg.op("dve", lambda e: e.tensor_tensor(o_tm[:, hs, :], dtmp[:, :, :], o1[:, :, :], op=ALU.add), reads=[o1, dtmp], writes=[o_tm])
        yield

    def delta_chunk(ci, state_only, fill=iter(())):
        tok0 = ci * 64
        beta_g(tok0)
        tm_from_fm(kc, k_tm, tok0)
        tm_from_fm(vc, v_tm, tok0)
        pg.op("dve", lambda e: e.tensor_tensor(sq[:, :, :], k_tm[:, :, :], k_tm[:, :, :], op=ALU.mult), reads=[k_tm], writes=[sq])
        pg.op("dve", lambda e: e.tensor_reduce(ssk[:, :], sq[:, :, :], axis=AX.X, op=ALU.add), reads=[sq], writes=[ssk])
        pg.op("act", lambda e: e.activation(rk[:, :], ssk[:, :], AF.Ln, bias=EPS), reads=[ssk], writes=[rk])
        pg.op("act", lambda e: e.activation(rk[:, :], rk[:, :], AF.Exp, scale=-0.5), reads=[rk], writes=[rk])
        pg.op("dve", lambda e: e.tensor_tensor(k_n[:, :, :], k_tm[:, :, :], bc(rk[:, :].unsqueeze(2), [64, H, 128]), op=ALU.mult), reads=[k_tm, rk], writes=[k_n])
        for g in range(NHG):
            ps = PS()
            pv = psb16(ps)

            def f(e, g=g, pv=pv):
                for j in range(HG):
                    r = e.transpose(pv[:, j * 64:(j + 1) * 64], k_n[:, g * HG + j, :], identb[0:64, 0:64])
                return r
            pg.op("pe", f, reads=[k_n, identb], writes=[ps])
            pg.op("act", lambda e, g=g, pv=pv: e.activation(k_nT[:, g * HG:(g + 1) * HG, :], r3(pv[:, 0:HG * 64], HG), AF.Copy), reads=[ps], writes=[k_nT])
        if not state_only:
            pg.op("act", lambda e: e.activation(sqT[:, :, :], qc[:, :, tok0:tok0 + 64], AF.Square), reads=[qc], writes=[sqT])
            ps = PS()

            def f(e, ps=ps):
                for h in range(H):
                    r = e.matmul(ps.ap[0:64, h:h + 1], sqT[:, h, :], ONES[:, 0:1], start=True, stop=True)
                return r
            pg.op("pe", f, reads=[sqT, cst], writes=[ps])
            pg.op("act", lambda e, ps=ps: e.activation(rq[:, :], ps.ap[0:64, 0:H], AF.Ln, bias=EPS), reads=[ps], writes=[rq])
            pg.op("act", lambda e: e.activation(rq[:, :], rq[:, :], AF.Exp, scale=-0.5), reads=[rq], writes=[rq])
            pg.op("dve", lambda e: e.tensor_scalar(rq[:, :], rq[:, :], 128.0 ** -0.5, None, op0=ALU.mult), reads=[rq], writes=[rq])
        gens = [chain(g, tok0, state_only) for g in range(NHG)]
        alive = list(gens)
        while alive:
            for gen in list(alive):
                try:
                    next(gen)
                except StopIteration:
                    alive.remove(gen)
            next(fill, None)
        if not state_only:
            pg.op("dve", lambda e: e.tensor_tensor(sq[:, :, :], o_tm[:, :, :], o_tm[:, :, :], op=ALU.mult), reads=[o_tm], writes=[sq])
            pg.op("dve", lambda e: e.tensor_reduce(ssk[:, :], sq[:, :, :], axis=AX.X, op=ALU.add), reads=[sq], writes=[ssk])
            pg.op("act", lambda e: e.activation(ssk[:, :], ssk[:, :], AF.Ln, scale=1.0 / 128, bias=EPS), reads=[ssk], writes=[ssk])
            pg.op("act", lambda e: e.activation(ssk[:, :], ssk[:, :], AF.Exp, scale=-0.5), reads=[ssk], writes=[ssk])
            pg.op("dve", lambda e: e.tensor_tensor(o_tm[:, :, :], o_tm[:, :, :], bc(ssk[:, :].unsqueeze(2), [64, H, 128]), op=ALU.mult), reads=[o_tm, ssk], writes=[o_tm])
            pg.op("dve", lambda e: e.tensor_tensor(o_tm[:, :, :], o_tm[:, :, :], bc(g_dn[0:64, :].unsqueeze(1), [64, H, 128]), op=ALU.mult), reads=[o_tm, g_dn], writes=[o_tm])
            zs = z_tm[ci]
            pg.op("act", lambda e: e.activation(sq[:, :, :], r3(zs[:, :], H), AF.Silu), reads=[zs], writes=[sq])
            pg.op("dve", lambda e: e.tensor_tensor(ydn[:, :, :], o_tm[:, :, :], sq[:, :, :], op=ALU.mult), reads=[o_tm, sq], writes=[ydn])
            ps = PS()
            psv = psb16(ps)

            def f(e, psv=psv):
                for h in range(H):
                    r = e.transpose(psv[:, h * 64:(h + 1) * 64], ydn[:, h, :], identb[0:64, 0:64])
                return r
            pg.op("pe", f, reads=[ydn, identb], writes=[ps])
            pg.op("act", lambda e, psv=psv: e.activation(ydnT[:, :, tok0:tok0 + 64], r3(psv[:, 0:H * 64], H), AF.Copy), reads=[ps], writes=[ydnT])

    def router_and_scatter(hb, st):
        rms_rstd(hb, hb[:, :], st1[:, 1:2], n2b)
        pg.op("act", lambda e: e.activation(n2[:, :], hb[:, :], AF.Copy, scale=st1[:, 1:2]), reads=[hb, st1], writes=[n2])
        pg.op("act", lambda e: e.activation(n2b[:, :], n2[:, :], AF.Copy), reads=[n2], writes=[n2b])
        for c0 in range(0, KD, 4):
            nb = min(4, KD - c0)
            ps = PS()

            def f(e, c0=c0, ps=ps, nb=nb):
                for j in range(nb):
                    r = e.transpose(ps.ap[:, j * 128:(j + 1) * 128], n2[:, (c0 + j) * 128:(c0 + j + 1) * 128], ident)
                return r
            pg.op("pe", f, reads=[n2, cst], writes=[ps])
            pg.op("dve", lambda e, c0=c0, ps=ps, nb=nb: e.tensor_tensor(n2T[:, c0:c0 + nb, :], ps.ap[:, 0:nb * 128].rearrange("p (c t) -> p c t", c=nb),
                                                                     bc(gmoeT[:, c0:c0 + nb].unsqueeze(2), [128, nb, 128]), op=ALU.mult), reads=[ps, gmoeT], writes=[n2T])
        NL = c.NGR + NE
        ps = PS()

        def f(e, ps=ps):
            for cc in range(KD):
                r = e.matmul(ps.ap[:, 0:NL], n2T[:, cc, :], wr[:, cc, :], start=(cc == 0), stop=(cc == KD - 1))
            return r
        pg.op("pe", f, reads=[n2T, wr], writes=[ps])
        pg.op("dve", lambda e, ps=ps: e.tensor_tensor(lg[:, :], ps.ap[:, 0:NL], brep[:, :], op=ALU.add), reads=[ps, brep], writes=[lg])
        G = c.NGR; E = c.EPG
        pg.op("dve", lambda e: e.tensor_reduce(rt[:, 0:1], lg[:, 0:G], axis=AX.X, op=ALU.max), reads=[lg], writes=[rt])
        pg.op("dve", lambda e: e.tensor_scalar(ohg[:, :], lg[:, 0:G], rt[:, 0:1], None, op0=ALU.is_equal), reads=[lg, rt], writes=[ohg])
        pg.op("dve", lambda e: e.tensor_scalar(rt[:, 8:8 + G], lg[:, 0:G], rt[:, 0:1], None, op0=ALU.subtract), reads=[lg, rt], writes=[rt])
        pg.op("act", lambda e: e.activation(rt[:, 8:8 + G], rt[:, 8:8 + G], AF.Exp), reads=[rt], writes=[rt])
        pg.op("dve", lambda e: e.tensor_reduce(rt[:, 1:2], rt[:, 8:8 + G], axis=AX.X, op=ALU.add), reads=[rt], writes=[rt])
        pg.op("dve", lambda e: e.reciprocal(rt[:, 1:2], rt[:, 1:2]), reads=[rt], writes=[rt])
        pg.op("dve", lambda e: e.tensor_tensor(tmpe[:, :].rearrange("p (g x) -> p g x", g=G), lg[:, G:].rearrange("p (g x) -> p g x", g=G),
                                               bc(ohg[:, :].unsqueeze(2), [128, G, E]), op=ALU.mult), reads=[lg, ohg], writes=[tmpe])
        pg.op("dve", lambda e: e.tensor_reduce(le[:, :], tmpe[:, :].rearrange("p (g x) -> p x g", g=G), axis=AX.X, op=ALU.add), reads=[tmpe], writes=[le])
        pg.op("dve", lambda e: e.tensor_reduce(rt[:, 2:3], le[:, :], axis=AX.X, op=ALU.max), reads=[le], writes=[rt])
        pg.op("dve", lambda e: e.tensor_scalar(oh1[:, :], le[:, :], rt[:, 2:3], None, op0=ALU.is_equal), reads=[le, rt], writes=[oh1])
        pg.op("dve", lambda e: e.scalar_tensor_tensor(le[:, :], oh1[:, :], NEG, le[:, :], op0=ALU.mult, op1=ALU.add), reads=[oh1, le], writes=[le])
        pg.op("dve", lambda e: e.tensor_reduce(rt[:, 3:4], le[:, :], axis=AX.X, op=ALU.max), reads=[le], writes=[rt])
        pg.op("dve", lambda e: e.tensor_scalar(oh2[:, :], le[:, :], rt[:, 3:4], None, op0=ALU.is_equal), reads=[le, rt], writes=[oh2])
        pg.op("dve", lambda e: e.tensor_tensor(rt[:, 4:5], rt[:, 2:3], rt[:, 3:4], op=ALU.subtract), reads=[rt], writes=[rt])
        pg.op("act", lambda e: e.activation(rt[:, 4:5], rt[:, 4:5], AF.Sigmoid), reads=[rt], writes=[rt])
        pg.op("dve", lambda e: e.tensor_tensor(wts[:, st, 0:1], rt[:, 4:5], rt[:, 1:2], op=ALU.mult), reads=[rt], writes=[wts])
        pg.op("dve", lambda e: e.tensor_tensor(wts[:, st, 1:2], rt[:, 1:2], wts[:, st, 0:1], op=ALU.subtract), reads=[rt, wts], writes=[wts])
        for k, ohk in enumerate((oh1, oh2)):
            pg.op("dve", lambda e, k=k, ohk=ohk: e.tensor_tensor(oh[:, k, :].rearrange("p (g x) -> p g x", g=G), bc(ohg[:, :].unsqueeze(2), [128, G, E]),
                                                                 bc(ohk[:, :].unsqueeze(1), [128, G, E]), op=ALU.mult), reads=[ohg, ohk], writes=[oh])
        pg.op("dve", lambda e: e.tensor_tensor(ohs[:, :], oh[:, 0, :], oh[:, 1, :], op=ALU.add), reads=[oh], writes=[ohs])
        ps = PS()

        def f(e, ps=ps):
            e.matmul(ps.ap[:, 0:NE], sltb[:, :], ohs[:, :], start=True, stop=True)
            return e.matmul(ps.ap[:, 64:64 + NE], onesb[:, :], ohs[:, :], start=True, stop=True)
        pg.op("pe", f, reads=[sltb, onesb, ohs], writes=[ps])
        pg.op("dve", lambda e, ps=ps: e.tensor_tensor(posf[:, :], ps.ap[:, 0:NE], cnt_bc[:, :], op=ALU.add), reads=[ps, cnt_bc], writes=[posf])
        pg.op("dve", lambda e: e.tensor_tensor(posf[:, :], posf[:, :], IOTA_E[:, 0:NE], op=ALU.add), reads=[posf, cst], writes=[posf])
        pg.op("dve", lambda e, ps=ps: e.tensor_tensor(cnt_bc[:, :], cnt_bc[:, :], ps.ap[:, 64:64 + NE], op=ALU.add), reads=[ps, cnt_bc], writes=[cnt_bc])
        for k in range(2):
            pg.op("dve", lambda e, k=k: e.tensor_tensor(tmpe[:, :], posf[:, :], oh[:, k, :], op=ALU.mult), reads=[posf, oh], writes=[tmpe])
            pg.op("dve", lambda e, k=k: e.tensor_reduce(slf[:, k:k + 1], tmpe[:, :], axis=AX.X, op=ALU.add), reads=[tmpe], writes=[slf])
        pg.op("dve", lambda e: e.tensor_copy(slot_i[:, st, :], slf[:, :]), reads=[slf], writes=[slot_i])
        for k in range(2):
            pg.dma("pool", lambda e, k=k: e.indirect_dma_start(out=xe_d, out_offset=bass.IndirectOffsetOnAxis(ap=slot_i[:, st, k:k + 1], axis=0),
                                                               in_=n2b[:, :], in_offset=None), reads=[n2b, slot_i], writes=[])

    def block(xsrc_d, blk, state_only, warm=False):
        n = T
        for s in range(NSUB):
            make_n1T(xsrc_d, blk * T + s * 128, s)
        if not state_only:
            for g0 in range(0, DP, 512):
                ncol = min(512, DP - g0)
                wt, wv = load_w(f"in{c.o_pool + g0}", w_in_T(c.o_pool + g0, ncol), KD, ncol)
                for s in range(NSUB):
                    ps = PS()
                    mm_tm(ps, n1T, lambda cc, s=s: n1T[:, cc, s * 128:(s + 1) * 128], wt, wv, KD, ncol)
                    pg.op("act", lambda e, ps=ps, s=s, g0=g0, ncol=ncol: e.activation(u_tm[s][:, g0:g0 + ncol], ps.ap[:, 0:ncol], AF.Copy), reads=[ps], writes=[u_tm[s]])
            for ct in range(DP // 128):
                g = ct // GK
                for s in range(NSUB):
                    ps = PS()
                    prev = u_prev if s == 0 else u_tm[s - 1]
                    first = (blk == 0 and s == 0)

                    def f(e, ps=ps, prev=prev, s=s, g=g, ct=ct, first=first):
                        e.matmul(ps.ap[:, 0:128], prev[:, ct * 128:(ct + 1) * 128], bandb[:, g, 0, :], start=True, stop=False)
                        return e.matmul(ps.ap[:, 0:128], u_tm[s][:, ct * 128:(ct + 1) * 128], bandb[:, g, 2 if first else 1, :], start=False, stop=True)
                    pg.op("pe", f, reads=[prev, u_tm[s], bandb], writes=[ps])
                    pg.op("act", lambda e, ps=ps, s=s, ct=ct: e.activation(dT[:, ct, s * 128:(s + 1) * 128], ps.ap[:, 0:128], AF.Copy), reads=[ps], writes=[dT])
            pg.op("dve", lambda e: e.tensor_copy(u_prev[:, :], u_tm[NSUB - 1][:, :]), reads=[u_tm[NSUB - 1]], writes=[u_prev])
            for g in range(4):
                for oc in range(GK):
                    ps = PS()

                    def f(e, ps=ps, g=g, oc=oc):
                        for kk in range(GK):
                            r = e.matmul(ps.ap[:, 0:n], wgrp[:, g, kk, oc * 128:(oc + 1) * 128], dT[:, g * GK + kk, :], start=(kk == 0), stop=(kk == GK - 1))
                        return r
                    pg.op("pe", f, reads=[wgrp, dT], writes=[ps])
                    ot = g * GK + oc
                    pg.op("act", lambda e, ps=ps, ot=ot: e.activation(ypT[:, ot, :], ps.ap[:, 0:n], AF.Copy, scale=pscaleT[:, ot:ot + 1]), reads=[ps, pscaleT], writes=[ypT])
            for g0 in range(0, DQK, 512):
                ncol = min(512, DQK - g0)
                wt, wv = load_w(f"in{c.o_z + g0}", w_in_T(c.o_z + g0, ncol), KD, ncol)
                for ci in range(NCH):
                    ps = PS()

                    def f(e, ps=ps, ci=ci, wv=wv, ncol=ncol):
                        for cc in range(KD):
                            r = e.matmul(ps.ap[0:64, 0:ncol], n1T[:, cc, ci * 64:(ci + 1) * 64], wv[:, cc, 0:ncol], start=(cc == 0), stop=(cc == KD - 1))
                        return r
                    pg.op("pe", f, reads=[wt, n1T], writes=[ps])
                    pg.op("act", lambda e, ps=ps, ci=ci, g0=g0, ncol=ncol: e.activation(z_tm[ci][:, g0:g0 + ncol], ps.ap[0:64, 0:ncol], AF.Copy), reads=[ps], writes=[z_tm[ci]])
            proj_qkv(0, n)
        proj_qkv(1, n)
        proj_qkv(2, n)
        def filler():
            for g0 in range(0, D, 512):
                ncol = min(512, D - g0)
                wtu, wvu = load_w(f"upp{g0}", w_up_pool_d[:, g0:g0 + ncol], DP // 128, ncol)
                wtg, wvg = load_w(f"in{c.o_gp + g0}", w_in_T(c.o_gp + g0, ncol), KD, ncol)
                for j in range(ncol // 128):
                    psa = PSP('f', [6, 7]); psg = PSP('f', [6, 7])
                    mm_fm(psa, wtu, wvu, j * 128, ypT, lambda cc: ypT[:, cc, :], DP // 128, n)
                    mm_fm(psg, wtg, wvg, j * 128, n1T, lambda cc: n1T[:, cc, :], KD, n)
                    pg.op("act", lambda e, psg=psg: e.activation(sg[:, 0:n], psg.ap[:, 0:n], AF.Sigmoid), reads=[psg], writes=[sg])
                    ot = (g0 + j * 128) // 128
                    pg.op("dve", lambda e, psa=psa, ot=ot: e.tensor_tensor(mT[:, ot, :], psa.ap[:, 0:n], sg[:, 0:n], op=ALU.mult), reads=[psa, sg], writes=[mT])
                    yield
            for g0 in range(0, D, 512):
                ncol = min(512, D - g0)
                wtg, wvg = load_w(f"in{c.o_gd + g0}", w_in_T(c.o_gd + g0, ncol), KD, ncol)
                for j in range(ncol // 128):
                    psg = PSP('f', [6, 7])
                    mm_fm(psg, wtg, wvg, j * 128, n1T, lambda cc: n1T[:, cc, :], KD, n)
                    ot = (g0 + j * 128) // 128
                    pg.op("act", lambda e, psg=psg, ot=ot: e.activation(sgd[:, ot, :], psg.ap[:, 0:n], AF.Sigmoid), reads=[psg], writes=[sgd])
                    yield

        fill = iter(()) if state_only else filler()
        for ci in range(NCH):
            delta_chunk(ci, state_only, fill)
        for _ in fill:
            pass
        if state_only:
            return
        for g0 in range(0, D, 512):
            ncol = min(512, D - g0)
            wtu, wvu = load_w(f"upd{g0}", w_up_dn_d[:, g0:g0 + ncol], DQK // 128, ncol)
            for j in range(ncol // 128):
                psa = PS()
                mm_fm(psa, wtu, wvu, j * 128, ydnT, lambda cc: ydnT[:, cc, :], DQK // 128, n)
                ot = (g0 + j * 128) // 128
                pg.op("dve", lambda e, psa=psa, ot=ot: e.tensor_tensor(sg[:, 0:n], psa.ap[:, 0:n], sgd[:, ot, :], op=ALU.mult), reads=[psa, sgd], writes=[sg])
                pg.op("dve", lambda e, ot=ot: e.tensor_tensor(mT[:, ot, :], mT[:, ot, :], sg[:, 0:n], op=ALU.add), reads=[mT, sg], writes=[mT])
        assert NSUB <= 2
        for s in range(NSUB):
            hb = xt[s % 2]
            row0 = blk * T + s * 128
            pg.dma("sp", lambda e, hb=hb, row0=row0: e.dma_start(out=hb[:, :], in_=xsrc_d[row0:row0 + 128, :]), writes=[hb])
        for g0 in range(0, D, 512):
            ncol = min(512, D - g0)
            wt, wv = load_w(f"out{g0}", w_out_d[:, g0:g0 + ncol], KD, ncol)
            for s in range(NSUB):
                hb = xt[s % 2]
                ps = PS()
                mm_tm(ps, mT, lambda cc, s=s: mT[:, cc, s * 128:(s + 1) * 128], wt, wv, KD, ncol)
                pg.op("dve", lambda e, ps=ps, hb=hb, g0=g0, ncol=ncol: e.tensor_tensor(hb[:, g0:g0 + ncol], hb[:, g0:g0 + ncol], ps.ap[:, 0:ncol], op=ALU.add),
                      reads=[ps, hb], writes=[hb])
        for s in range(NSUB):
            hb = xt[s % 2]
            row0 = blk * T + s * 128
            pg.dma("sp", lambda e, hb=hb, row0=row0: e.dma_start(out=h1s_d[row0:row0 + 128, :], in_=hb[:, :]), reads=[hb], writes=[])
            router_and_scatter(hb, blk * NSUB + s)

    if prefix:
        for blk in range(NBLK):
            block(xp_d, blk, True)
    make_n1T(xw_d, 0, 0)
    for g0 in range(0, DP, 512):
        ncol = min(512, DP - g0)
        wt, wv = load_w(f"in{c.o_pool + g0}", w_in_T(c.o_pool + g0, ncol), KD, ncol)
        ps = PS()
        mm_tm(ps, n1T, lambda cc: n1T[:, cc, 0:128], wt, wv, KD, ncol)
        pg.op("act", lambda e, ps=ps, g0=g0, ncol=ncol: e.activation(u_prev[:, g0:g0 + ncol], ps.ap[:, 0:ncol], AF.Copy), reads=[ps], writes=[u_prev])
    for g0 in range(0, DQK, 512):
        ncol = min(512, DQK - g0)
        wt, wv = load_w(f"in{c.o_q + g0}", w_in_T(c.o_q + g0, ncol), KD, ncol)
        for j in range(ncol // 128):
            ps = PS()
            mm_fm(ps, wt, wv, j * 128, n1T, lambda cc: n1T[:, cc, 0:128], KD, 128)
            hh = (g0 + j * 128) // 128
            pg.op("act", lambda e, ps=ps, hh=hh: e.activation(halo[:, hh, :], ps.ap[:, 125:128], AF.Copy), reads=[ps], writes=[halo])
    if not prefix:
        pass
    pg.wait_tokens("pool", zero_toks)
    for blk in range(NBLK):
        block(x_d, blk, False)

    pg.barrier()
    aoff[0] = phase_mark
    xe_sb = sb("xe_sb", [128, NCAPT, D], BF16)
    xeT = sb("xeT", [128, KD, CAP], BF16)
    hmT = sb("hmT", [128, c.DE // 128, CAP], BF16)
    sgm = sb("sgm", [128, CAP])
    yb = [sb(f"yb{i}", [128, D]) for i in range(2)]
    for ex in range(NE):
        pg.dma("sp", lambda e, ex=ex: e.dma_start(out=xe_sb[:, :, :], in_=xe_d[ex * CAP:(ex + 1) * CAP, :].rearrange("(t p) d -> p t d", p=128)), writes=[xe_sb])
        for t in range(NCAPT):
            for c0 in range(0, KD, 8):
                nb = min(8, KD - c0)
                ps = PS()
                psv = ps.ap[:, :].bitcast(BF16)

                def f(e, t=t, c0=c0, nb=nb, psv=psv):
                    for j in range(nb):
                        r = e.transpose(psv[:, j * 128:(j + 1) * 128], xe_sb[:, t, (c0 + j) * 128:(c0 + j + 1) * 128], identb[:, :])
                    return r
                pg.op("pe", f, reads=[xe_sb, identb], writes=[ps])
                pg.op("dve", lambda e, t=t, c0=c0, nb=nb, psv=psv: e.tensor_tensor(xeT[:, c0:c0 + nb, t * 128:(t + 1) * 128], psv[:, 0:nb * 128].rearrange("p (c t) -> p c t", c=nb),
                                                                                 bc(gmoeT[:, c0:c0 + nb].unsqueeze(2), [128, nb, 128]), op=ALU.mult), reads=[ps, gmoeT], writes=[xeT])
        wtg, wvg = load_w_cast(w_gate_d[ex], KD, c.DE)
        wtu, wvu = load_w_cast(w_upe_d[ex], KD, c.DE)
        for ft in range(c.DE // 128):
            psg = PS(); psu = PS()
            mm_fm(psg, wtg, wvg, ft * 128, xeT, lambda cc: xeT[:, cc, :], KD, CAP)
            mm_fm(psu, wtu, wvu, ft * 128, xeT, lambda cc: xeT[:, cc, :], KD, CAP)
            pg.op("act", lambda e, psg=psg: e.activation(sgm[:, :], psg.ap[:, 0:CAP], AF.Silu), reads=[psg], writes=[sgm])
            pg.op("dve", lambda e, psu=psu, ft=ft: e.tensor_tensor(hmT[:, ft, :], psu.ap[:, 0:CAP], sgm[:, :], op=ALU.mult), reads=[psu, sgm], writes=[hmT])
        FK = c.DE // 128
        t_d, wvd = load_w_cast(w_down_d[ex], FK, D)
        for t in range(NCAPT):
            ybt = yb[t % 2]
            for g0 in range(0, D, 512):
                ncol = min(512, D - g0)
                ps = PS()

                def f(e, ps=ps, t=t, g0=g0, ncol=ncol, wvd=wvd):
                    for kk in range(FK):
                        r = e.matmul(ps.ap[:, 0:ncol], hmT[:, kk, t * 128:(t + 1) * 128], wvd[:, kk, g0:g0 + ncol], start=(kk == 0), stop=(kk == FK - 1))
                    return r
                pg.op("pe", f, reads=[hmT, t_d], writes=[ps])
                pg.op("act", lambda e, ps=ps, ybt=ybt, g0=g0, ncol=ncol: e.activation(ybt[:, g0:g0 + ncol], ps.ap[:, 0:ncol], AF.Copy), reads=[ps], writes=[ybt])
            r0 = ex * CAP + t * 128
            pg.dma("sp", lambda e, ybt=ybt, r0=r0: e.dma_start(out=ye_d[r0:r0 + 128, :], in_=ybt[:, :]), reads=[ybt], writes=[])

    pg.barrier()
    aoff[0] = phase_mark
    wpp = sb("wpp", [128, c.DPLE // 128, D], BF16)
    pg.dma("pool", lambda e: e.dma_start(out=wpp[:, :, :], in_=w_pp_d.rearrange("(c p) n -> p c n", p=128)), writes=[wpp])
    g_fin = load_rep("g_fin", norm_final_d, D)
    y1 = [sb(f"y1_{i}", [128, D]) for i in range(2)]
    y2 = [sb(f"y2_{i}", [128, D]) for i in range(2)]
    h2 = [sb(f"h2_{i}", [128, D]) for i in range(NSUB)]
    pt = sb("pt", [128, c.DPLE]); ptb = sb("ptb", [128, c.DPLE], BF16)
    pT = sb("pT", [128, c.DPLE // 128, T], BF16)
    PK = c.DPLE // 128
    for blk in range(NBLK):
        for s in range(NSUB):
            st = blk * NSUB + s
            row0 = st * 128
            hb = h2[s]
            pg.dma("sp", lambda e, hb=hb, row0=row0: e.dma_start(out=hb[:, :], in_=h1s_d[row0:row0 + 128, :]), writes=[hb])
            ya, yb2 = y1[st % 2], y2[st % 2]
            pg.dma("pool", lambda e, ya=ya, st=st: e.indirect_dma_start(out=ya[:, :], out_offset=None, in_=ye_d,
                                                                        in_offset=bass.IndirectOffsetOnAxis(ap=slot_i[:, st, 0:1], axis=0)), reads=[slot_i], writes=[ya])
            pg.dma("pool", lambda e, yb2=yb2, st=st: e.indirect_dma_start(out=yb2[:, :], out_offset=None, in_=ye_d,
                                                                          in_offset=bass.IndirectOffsetOnAxis(ap=slot_i[:, st, 1:2], axis=0)), reads=[slot_i], writes=[yb2])
            pg.op("dve", lambda e, hb=hb, ya=ya, st=st: e.scalar_tensor_tensor(hb[:, :], ya[:, :], wts[:, st, 0:1], hb[:, :], op0=ALU.mult, op1=ALU.add), reads=[ya, wts, hb], writes=[hb])
            pg.op("dve", lambda e, hb=hb, yb2=yb2, st=st: e.scalar_tensor_tensor(hb[:, :], yb2[:, :], wts[:, st, 1:2], hb[:, :], op0=ALU.mult, op1=ALU.add), reads=[yb2, wts, hb], writes=[hb])
            rms_rstd(hb, hb[:, :], st1[:, 2:3], n2b)
            pg.op("act", lambda e, hb=hb: e.activation(n2b[:, :], hb[:, :], AF.Copy, scale=st1[:, 2:3]), reads=[hb, st1], writes=[n2b])
            for c0 in range(0, KD, 8):
                nb = min(8, KD - c0)
                ps = PS()
                psv = ps.ap[:, :].bitcast(BF16)

                def f(e, c0=c0, nb=nb, psv=psv):
                    for j in range(nb):
                        r = e.transpose(psv[:, j * 128:(j + 1) * 128], n2b[:, (c0 + j) * 128:(c0 + j + 1) * 128], identb[:, :])
                    return r
                pg.op("pe", f, reads=[n2b, identb], writes=[ps])
                pg.op("dve", lambda e, c0=c0, nb=nb, psv=psv, s=s: e.tensor_tensor(n1T[:, c0:c0 + nb, s * 128:(s + 1) * 128], psv[:, 0:nb * 128].rearrange("p (c t) -> p c t", c=nb),
                                                                                 bc(gpleT[:, c0:c0 + nb].unsqueeze(2), [128, nb, 128]), op=ALU.mult), reads=[ps, gpleT], writes=[n1T])
            pg.dma("sp", lambda e, row0=row0: e.dma_start(out=pt[:, :], in_=p_d[row0:row0 + 128, :]), writes=[pt])
            pg.op("act", lambda e: e.activation(ptb[:, :], pt[:, :], AF.Copy), reads=[pt], writes=[ptb])
            ps = PS()
            psv = ps.ap[:, :].bitcast(BF16)

            def f(e, psv=psv):
                for j in range(PK):
                    r = e.transpose(psv[:, j * 128:(j + 1) * 128], ptb[:, j * 128:(j + 1) * 128], identb[:, :])
                return r
            pg.op("pe", f, reads=[ptb, identb], writes=[ps])
            pg.op("act", lambda e, psv=psv, s=s: e.activation(pT[:, :, s * 128:(s + 1) * 128], psv[:, 0:PK * 128].rearrange("p (c t) -> p c t", c=PK), AF.Copy), reads=[ps], writes=[pT])
        for g0 in range(0, D, 512):
            ncol = min(512, D - g0)
            wt, wv = load_w(f"pg{g0}", w_pg_d[:, g0:g0 + ncol], KD, ncol)
            for s in range(NSUB):
                hb = h2[s]
                psg = PS(); psp = PS()
                mm_tm(psg, n1T, lambda cc, s=s: n1T[:, cc, s * 128:(s + 1) * 128], wt, wv, KD, ncol)

                def f(e, psp=psp, s=s, g0=g0, ncol=ncol):
                    for kk in range(PK):
                        r = e.matmul(psp.ap[:, 0:ncol], pT[:, kk, s * 128:(s + 1) * 128], wpp[:, kk, g0:g0 + ncol], start=(kk == 0), stop=(kk == PK - 1))
                    return r
                pg.op("pe", f, reads=[pT, wpp], writes=[psp])
                pg.op("act", lambda e, psg=psg, ncol=ncol: e.activation(sg[:, 0:ncol], psg.ap[:, 0:ncol], AF.Sigmoid), reads=[psg], writes=[sg])
                pg.op("dve", lambda e, psp=psp, ncol=ncol: e.tensor_tensor(sg[:, 0:ncol], sg[:, 0:ncol], psp.ap[:, 0:ncol], op=ALU.mult), reads=[psp, sg], writes=[sg])
                pg.op("dve", lambda e, hb=hb, g0=g0, ncol=ncol: e.tensor_tensor(hb[:, g0:g0 + ncol], hb[:, g0:g0 + ncol], sg[:, 0:ncol], op=ALU.add), reads=[hb, sg], writes=[hb])
        for s in range(NSUB):
            hb = h2[s]
            row0 = (blk * NSUB + s) * 128
            rms_rstd(hb, hb[:, :], st1[:, 3:4], n2b)
            pg.op("dve", lambda e, hb=hb: e.scalar_tensor_tensor(hb[:, :], hb[:, :], st1[:, 3:4], g_fin[:, :], op0=ALU.mult, op1=ALU.mult), reads=[hb, st1, g_fin], writes=[hb])
            pg.dma("sp", lambda e, hb=hb, row0=row0: e.dma_start(out=out_d[row0:row0 + 128, :], in_=hb[:, :]), reads=[hb], writes=[])

    build.stats = dict(arena_cols=amax[0], nsem=pg.nsem, cnt=dict(pg.cnt), lens={e: len(pg.lists[e]) for e in pg.ENGS})
    pg.finalize()
    es.close()
    return nc


def make_consts(cfg, first_half):
    cst = np.zeros((128, 1024), np.float32)
    cst[:, 0:128] = np.eye(128)
    m = np.arange(128)[:, None] % 64
    i = np.arange(64)[None, :]
    cst[:, 128:192] = (m <= i)
    cst[:, 192:256] = (m > i)
    cst[:, 256:320] = NEG * (m > i)
    cst[:, 320:384] = -1.0 * (m < i)
    cst[:, 384:448] = (m == i)
    cst[:, 448:576] = 1.0
    tp = np.arange(128)[:, None]
    t = np.arange(128)[None, :]
    cst[:, 576:704] = (tp < t)
    cst[:, 704:704 + cfg.NE] = (np.arange(cfg.NE) * cfg.CAP)[None, :]
    band = np.zeros((4, 3, 128, 128), np.float32)
    for g, w in enumerate(cfg.WINS):
        cur = ((tp <= t) & (tp > t - w)).astype(np.float32) / w - (tp == t)
        prev = ((tp - 128 <= t) & (tp - 128 > t - w)).astype(np.float32) / w
        cnt = np.minimum(t + 1, w).astype(np.float32)
        fst = ((tp <= t) & (tp > t - w)).astype(np.float32) / cnt - (tp == t)
        band[g, 0] = prev
        band[g, 1] = cur
        band[g, 2] = fst if first_half else cur
    return cst, band


_CACHE = {}


def kernel(**inputs):
    cfg = Cfg()
    x = np.asarray(inputs["x"])
    B, SEQ, D = x.shape
    ncores = 8
    half = SEQ // 2
    names = ["norm_mix", "w_in", "pool_w", "pool_scale", "conv_w", "a_log", "dt_bias", "dn_norm", "w_up_pool", "w_up_dn",
             "w_out", "norm_moe", "w_router_group", "b_router_group", "w_router_expert", "b_router_expert", "w_gate", "w_up",
             "w_down", "norm_ple", "w_ple_gate", "w_ple_proj"]
    shared = {n: np.ascontiguousarray(np.asarray(inputs[n])[0], dtype=np.float32) for n in names}
    shared["norm_final"] = np.ascontiguousarray(inputs["norm_final"], dtype=np.float32)
    p = np.asarray(inputs["p"])[0]
    in_maps = []
    zeros_pre = np.zeros((half, D), np.float32)
    for cidx in range(ncores):
        b, hf = divmod(cidx, 2)
        m = dict(shared)
        m["x"] = np.ascontiguousarray(x[b, hf * half:(hf + 1) * half])
        m["xpre"] = zeros_pre if hf == 0 else np.ascontiguousarray(x[b, 0:half])
        m["xwarm"] = np.zeros((128, D), np.float32) if hf == 0 else np.ascontiguousarray(x[b, half - 128:half])
        m["p"] = np.ascontiguousarray(p[b, hf * half:(hf + 1) * half])
        cst, band = make_consts(cfg, hf == 0)
        m["cst"] = cst
        m["band"] = band
        in_maps.append(m)
    if "nc" not in _CACHE:
        _CACHE["nc"] = build(cfg)
    res = run_bass_kernel_spmd(_CACHE["nc"], in_maps, core_ids=list(range(ncores)))
    out = np.zeros((B, SEQ, D), np.float32)
    for cidx in range(ncores):
        b, hf = divmod(cidx, 2)
        out[b, hf * half:(hf + 1) * half] = res.results[cidx]["out"]
    return out
```

```python
import numpy as np
import concourse.bass as bass
import concourse.mybir as mybir
from concourse.bass_utils import run_bass_kernel_spmd
from contextlib import ExitStack

F32 = mybir.dt.float32
BF16 = mybir.dt.bfloat16
I32 = mybir.dt.int32
AF = mybir.ActivationFunctionType
ALU = mybir.AluOpType
AX = mybir.AxisListType
SEM_MAX = 1800
NEG = -30000.0
EPS = 1e-6


class Cfg:
    def __init__(self, **kw):
        self.D = 2048; self.DP = 1024; self.H = 8; self.NGR = 4; self.EPG = 8; self.DE = 512
        self.DPLE = 256; self.TOK = 4096; self.T = 256; self.CAP = 384; self.WINS = (2, 4, 8, 16)
        self.__dict__.update(kw)
        self.GW = self.DP // 4
        self.DQK = self.H * 128
        self.DIN = self.DP + 4 * self.DQK + 2 * self.H + 2 * self.D
        self.NE = self.NGR * self.EPG
        self.KD = self.D // 128
        self.HG = min(4, self.H)
        self.o_pool = 0
        self.o_q = self.DP
        self.o_k = self.DP + self.DQK
        self.o_v = self.DP + 2 * self.DQK
        self.o_z = self.DP + 3 * self.DQK
        self.o_b = self.DP + 4 * self.DQK
        self.o_a = self.o_b + self.H
        self.o_gp = self.o_a + self.H
        self.o_gd = self.o_gp + self.D


class Tile:
    def __init__(self, name, ap, root=None):
        self.name = name
        self.ap = ap
        self.root = root.root if root is not None else self
        if root is None:
            self._last_w = None
            self._readers = []

    @property
    def last_w(self):
        return self.root._last_w

    @last_w.setter
    def last_w(self, v):
        self.root._last_w = v

    @property
    def readers(self):
        return self.root._readers

    @readers.setter
    def readers(self, v):
        self.root._readers = v

    def __getitem__(self, k):
        return self.ap[k]


class Prog:
    ENGS = ("pe", "act", "dve", "pool", "sp")

    def __init__(self, nc, es):
        self.nc = nc
        self.es = es
        self.lists = {e: [] for e in self.ENGS}
        self.cnt = {e: 0 for e in self.ENGS}
        self.esems = {e: [] for e in self.ENGS}
        self.waited = {e: {} for e in self.ENGS}
        self.nsem = 0
        self.dq = {"sp": [], "pool": [], "act": []}
        self.dq_i = {"sp": 0, "pool": 0, "act": 0}
        self.NDQ = 12
        self.all_dma_tokens = {}

    def new_sem(self, name):
        self.nsem += 1
        return self.es.enter_context(self.nc.semaphore(f"{name}_{self.nsem}"))

    def eng_token(self, eng):
        n = self.cnt[eng]
        ep, v = divmod(n, SEM_MAX)
        while len(self.esems[eng]) <= ep:
            self.esems[eng].append(self.new_sem(f"e_{eng}"))
        self.cnt[eng] = n + 1
        return ("e", eng, ep, v + 1)

    def _need(self, eng, toks):
        out = []
        w = self.waited[eng]
        for t in toks:
            if t is None:
                continue
            if t[0] == "e":
                _, pe, ep, v = t
                if pe == eng and eng == "pe":
                    continue
                key = ("e", pe)
                cur = w.get(key, (-1, 0))
                if (ep, v) <= cur:
                    continue
                w[key] = (ep, v)
                out.append((self.esems[pe][ep], v))
            else:
                _, sem, v, sid = t
                cur = w.get(sid, 0)
                if v <= cur:
                    continue
                w[sid] = v
                out.append((sem, v))
        return out

    def _deps(self, reads, writes):
        toks = []
        for t in reads:
            toks.append(t.last_w)
        for t in writes:
            toks.append(t.last_w)
            toks.extend(t.readers)
        return toks

    def op(self, eng, emit, reads=(), writes=()):
        waits = self._need(eng, self._deps(reads, writes))
        tok = self.eng_token(eng)
        sem = self.esems[eng][tok[2]]
        L = self.lists[eng]

        def run(e, waits=waits, emit=emit, sem=sem):
            for s, v in waits:
                e.wait_ge(s, v)
            emit(e).then_inc(sem, 1)
        L.append(run)
        for t in reads:
            t.readers.append(tok)
        for t in writes:
            t.last_w = tok
            t.readers = []
        return tok

    def dma(self, q, emit, reads=(), writes=()):
        lst = self.dq[q]
        i = self.dq_i[q]
        self.dq_i[q] = (i + 1) % self.NDQ
        if len(lst) <= i:
            lst.append([self.new_sem(f"d_{q}"), 0, None])
        ent = lst[i]
        if ent[1] + 16 > SEM_MAX:
            ent[0] = self.new_sem(f"d_{q}")
            ent[1] = 0
        prev_tok = ent[2]
        ent[1] += 16
        sem, val = ent[0], ent[1]
        tok = ("d", sem, val, id(sem))
        ent[2] = tok
        waits = self._need(q, self._deps(reads, writes) + [prev_tok])
        L = self.lists[q]

        def run(e, waits=waits, emit=emit, sem=sem):
            for s, v in waits:
                e.wait_ge(s, v)
            emit(e).then_inc(sem, 16)
        L.append(run)
        for t in reads:
            t.readers.append(tok)
        for t in writes:
            t.last_w = tok
            t.readers = []
        self.all_dma_tokens[id(sem)] = tok
        return tok

    def wait_tokens(self, eng, toks):
        waits = self._need(eng, toks)
        if not waits:
            return

        def run(e, waits=waits):
            for s, v in waits:
                e.wait_ge(s, v)
        self.lists[eng].append(run)

    def barrier(self):
        toks = list(self.all_dma_tokens.values())
        for e in self.ENGS:
            n = self.cnt[e]
            if n > 0:
                ep, v = divmod(n - 1, SEM_MAX)
                toks.append(("e", e, ep, v + 1))
        for e in self.ENGS:
            self.wait_tokens(e, toks)

    def finalize(self):
        final = list(self.all_dma_tokens.values())
        self.wait_tokens("sp", final)
        self.wait_tokens("pool", final)
        block = self.es.enter_context(self.nc.Block())
        lists = self.lists

        @block.tensor
        def _(e):
            for f in lists["pe"]:
                f(e)

        @block.scalar
        def _(e):
            for f in lists["act"]:
                f(e)

        @block.vector
        def _(e):
            for f in lists["dve"]:
                f(e)

        @block.gpsimd
        def _(e):
            for f in lists["pool"]:
                f(e)

        @block.sync
        def _(e):
            for f in lists["sp"]:
                f(e)


def bc(ap, shape):
    return ap.to_broadcast(list(shape))


def build(cfg, prefix=True):
    c = cfg
    D, DP, H, KD, T, TOK = c.D, c.DP, c.H, c.KD, c.T, c.TOK
    NSUB = T // 128
    NBLK = TOK // T
    HG = c.HG
    NHG = H // HG
    DQK = c.DQK
    NE, CAP = c.NE, c.CAP
    NCAPT = CAP // 128
    nc = bass.Bass("TRN2", target_bir_lowering=False)
    es = ExitStack()
    pg = Prog(nc, es)

    def din(name, shape, dt=F32):
        return nc.dram_tensor(name, list(shape), dt, kind="ExternalInput").ap()

    x_d = din("x", [TOK, D])
    xp_d = din("xpre", [TOK, D])
    xw_d = din("xwarm", [128, D])
    p_d = din("p", [TOK, c.DPLE])
    norm_mix_d = din("norm_mix", [D]); w_in_d = din("w_in", [D, c.DIN])
    pool_w_d = din("pool_w", [4, c.GW, c.GW]); pool_scale_d = din("pool_scale", [DP])
    conv_w_d = din("conv_w", [4, 3 * DQK]); a_log_d = din("a_log", [H]); dt_bias_d = din("dt_bias", [H])
    dn_norm_d = din("dn_norm", [128]); w_up_pool_d = din("w_up_pool", [DP, D]); w_up_dn_d = din("w_up_dn", [DQK, D])
    w_out_d = din("w_out", [D, D]); norm_moe_d = din("norm_moe", [D])
    w_rg_d = din("w_router_group", [D, c.NGR]); b_rg_d = din("b_router_group", [c.NGR])
    w_re_d = din("w_router_expert", [D, NE]); b_re_d = din("b_router_expert", [NE])
    w_gate_d = din("w_gate", [NE, D, c.DE]); w_upe_d = din("w_up", [NE, D, c.DE]); w_down_d = din("w_down", [NE, c.DE, D])
    norm_ple_d = din("norm_ple", [D]); w_pg_d = din("w_ple_gate", [D, D]); w_pp_d = din("w_ple_proj", [c.DPLE, D])
    norm_final_d = din("norm_final", [D])
    cst_d = din("cst", [128, 1024])
    band_d = din("band", [4, 3, 128, 128])
    out_d = nc.dram_tensor("out", [TOK, D], F32, kind="ExternalOutput").ap()
    h1s_d = nc.dram_tensor("h1s", [TOK, D], F32, kind="Internal").ap()
    xe_d = nc.dram_tensor("xe", [NE * CAP, D], BF16, kind="Internal").ap()
    ye_d = nc.dram_tensor("ye", [NE * CAP, D], F32, kind="Internal").ap()

    ARENA_COLS = 51 * 1024 + 512
    arena = es.enter_context(nc.sbuf_tensor("arena", [128, ARENA_COLS], F32))
    aoff = [0]
    amax = [0]

    def carve(off, shape, dt):
        free = 1
        for d in shape[1:]:
            free *= d
        ncol = free if dt == F32 or dt == I32 else (free + 1) // 2
        ap = arena[0:shape[0], off:off + ncol]
        if dt != F32:
            ap = ap.bitcast(dt)[:, 0:free]
        if len(shape) == 3:
            ap = ap.rearrange("p (a b) -> p a b", a=shape[1])
        elif len(shape) == 4:
            ap = ap.rearrange("p (a b c) -> p a b c", a=shape[1], b=shape[2])
        return ap, ncol

    def sb(name, shape, dt=F32):
        ap, ncol = carve(aoff[0], shape, dt)
        aoff[0] += ncol
        amax[0] = max(amax[0], aoff[0])
        assert aoff[0] <= ARENA_COLS, (name, aoff[0])
        return Tile(name, ap)

    def view(parent_tile, parent_off32, name, shape, dt=F32):
        ap, ncol = carve(parent_tile.base_off + parent_off32, shape, dt)
        return Tile(name, ap, root=parent_tile)

    def sb_b(name, shape, dt=F32):
        off = aoff[0]
        t = sb(name, shape, dt)
        t.base_off = off
        return t

    psb = [Tile(f"ps{i}", es.enter_context(nc.psum_tensor(f"ps{i}", [128, 512], F32))) for i in range(8)]
    ps_i = [0]

    def PS():
        t = psb[ps_i[0] % 8]
        ps_i[0] += 1
        return t

    pool_i = {}

    def PSP(key, banks):
        i = pool_i.get(key, 0)
        pool_i[key] = i + 1
        return psb[banks[i % len(banks)]]

    cst = sb("cst", [128, 1024])
    pg.dma("sp", lambda e: e.dma_start(out=cst[:, :], in_=cst_d), writes=[cst])
    ident = cst.ap[:, 0:128]
    U = cst.ap[:, 128:192]
    Bm = cst.ap[:, 192:256]
    NEGU = cst.ap[:, 256:320]
    SUN = cst.ap[:, 320:384]
    I64 = cst.ap[:, 384:448]
    ONES = cst.ap[:, 448:576]
    SLT = cst.ap[:, 576:704]
    IOTA_E = cst.ap[:, 704:704 + 32]
    identb = sb("identb", [128, 128], BF16)
    pg.op("dve", lambda e: e.tensor_copy(identb[:, :], ident), reads=[cst], writes=[identb])
    onesb = sb("onesb", [128, 128], BF16)
    pg.op("dve", lambda e: e.tensor_copy(onesb[:, :], ONES), reads=[cst], writes=[onesb])
    sltb = sb("sltb", [128, 128], BF16)
    pg.op("dve", lambda e: e.tensor_copy(sltb[:, :], SLT), reads=[cst], writes=[sltb])

    def load_col(name, d_ap, n, dt=F32):
        t = sb(name, [128, n // 128], dt)
        pg.dma("sp", lambda e: e.dma_start(out=t[:, :], in_=d_ap.rearrange("(c p) -> p c", p=128),
                                           allow_slow_non_contiguous=True), writes=[t])
        return t

    def load_rep(name, d_ap, n):
        t = sb(name, [128, n])
        pg.dma("sp", lambda e: e.dma_start(out=t[:, :], in_=d_ap.rearrange("(o n) -> o n", o=1).partition_broadcast(128)),
               writes=[t])
        return t

    gmixT = load_col("gmixT", norm_mix_d, D)
    pscaleT = load_col("pscaleT", pool_scale_d, DP)
    convT = sb("convT", [128, 4, 3 * DQK // 128])
    for k in range(4):
        pg.dma("sp", lambda e, k=k: e.dma_start(out=convT[:, k, :], in_=conv_w_d[k].rearrange("(c p) -> p c", p=128),
                                                allow_slow_non_contiguous=True), writes=[convT])
    gmoeT = load_col("gmoeT", norm_moe_d, D)
    gpleT = load_col("gpleT", norm_ple_d, D)
    g_dn = load_rep("g_dn", dn_norm_d, 128)
    brep = sb("brep", [128, c.NGR + NE])
    pg.dma("sp", lambda e: e.dma_start(out=brep[:, 0:c.NGR], in_=b_rg_d.rearrange("(o n) -> o n", o=1).partition_broadcast(128)), writes=[brep])
    pg.dma("sp", lambda e: e.dma_start(out=brep[:, c.NGR:], in_=b_re_d.rearrange("(o n) -> o n", o=1).partition_broadcast(128)), writes=[brep])
    hp = sb("hp", [128, 2 * H])
    pg.dma("sp", lambda e: e.dma_start(out=hp[:, 0:H], in_=a_log_d.rearrange("(o n) -> o n", o=1).partition_broadcast(128)), writes=[hp])
    pg.dma("sp", lambda e: e.dma_start(out=hp[:, H:], in_=dt_bias_d.rearrange("(o n) -> o n", o=1).partition_broadcast(128)), writes=[hp])
    pg.op("act", lambda e: e.activation(hp[:, 0:H], hp[:, 0:H], AF.Exp), reads=[hp], writes=[hp])
    pg.op("dve", lambda e: e.tensor_scalar(hp[:, 0:H], hp[:, 0:H], -1.0, None, op0=ALU.mult), reads=[hp], writes=[hp])
    wr = sb("wr", [128, KD, c.NGR + NE])
    pg.dma("sp", lambda e: e.dma_start(out=wr[:, :, 0:c.NGR], in_=w_rg_d.rearrange("(c p) n -> p c n", p=128), allow_slow_non_contiguous=True), writes=[wr])
    pg.dma("sp", lambda e: e.dma_start(out=wr[:, :, c.NGR:], in_=w_re_d.rearrange("(c p) n -> p c n", p=128), allow_slow_non_contiguous=True), writes=[wr])
    GK = c.GW // 128
    wgrp = sb("wgrp", [128, 4, GK, c.GW], BF16)
    for g in range(4):
        pg.dma("pool", lambda e, g=g: e.dma_start(out=wgrp[:, g, :, :], in_=pool_w_d[g].rearrange("(c p) n -> p c n", p=128)), writes=[wgrp])
    bandb = sb("bandb", [128, 4, 3, 128], BF16)
    pg.dma("pool", lambda e: e.dma_start(out=bandb[:, :, :, :], in_=band_d.rearrange("g k p n -> p g k n")), writes=[bandb])
    wba = sb("wba", [128, KD, 2 * H], BF16)
    pg.dma("pool", lambda e: e.dma_start(out=wba[:, :, :], in_=w_in_d[:, c.o_b:c.o_b + 2 * H].rearrange("(c p) n -> p c n", p=128)), writes=[wba])
    st1 = sb("st1", [128, 8])
    n1T = sb("n1T", [128, KD, T], BF16)
    sg = sb("sg", [128, 512])
    n2b = sb("n2b", [128, D], BF16)
    pg.op("dve", lambda e: e.memset(n2b[:, :], 0.0), writes=[n2b])
    zero_toks = []
    for r in range(NE * CAP // 128):
        zero_toks.append(pg.dma("sp", lambda e, r=r: e.dma_start(out=xe_d[r * 128:(r + 1) * 128, :], in_=n2b[:, :]), reads=[n2b]))

    NSLOT = 3
    WK = max(KD, DQK // 128, DP // 128)
    wslots = [sb(f"wslot{i}", [128, WK * 512], BF16) for i in range(NSLOT)]
    ws_i = [0]

    def load_w_cast(src2d, kchunks, ncols):
        t = wslots[ws_i[0] % NSLOT]
        ws_i[0] += 1
        view_ = t.ap[:, 0:kchunks * ncols].rearrange("p (c n) -> p c n", c=kchunks)
        pg.dma("pool", lambda e: e.dma_start(out=view_, in_=src2d.rearrange("(c p) n -> p c n", p=128)), writes=[t])
        return t, view_

    wscr = {}

    def prep_w(key, src2d, kchunks, ncols):
        if key in wscr:
            return
        d = nc.dram_tensor(f"wsc_{key}", [128, kchunks * ncols], BF16, kind="Internal").ap()
        tl = Tile(f"wsc_{key}", d)
        pg.dma("pool", lambda e: e.dma_start(out=d.rearrange("p (c n) -> p c n", c=kchunks), in_=src2d.rearrange("(c p) n -> p c n", p=128)), writes=[tl])
        wscr[key] = (tl, d)

    def load_w(key, src2d, kchunks, ncols):
        prep_w(key, src2d, kchunks, ncols)
        tl, d = wscr[key]
        t = wslots[ws_i[0] % NSLOT]
        ws_i[0] += 1
        view_ = t.ap[:, 0:kchunks * ncols].rearrange("p (c n) -> p c n", c=kchunks)
        pg.dma("sp", lambda e: e.dma_start(out=t.ap[:, 0:kchunks * ncols], in_=d), reads=[tl], writes=[t])
        return t, view_

    def groups(total):
        return [(g0, min(512, total - g0)) for g0 in range(0, total, 512)]

    for off_, key_ in ((c.o_k, "k"), (c.o_v, "v")):
        for g0, ncol in groups(DQK):
            prep_w(f"in{off_ + g0}", w_in_d[:, off_ + g0:off_ + g0 + ncol], KD, ncol)
    for g0, ncol in groups(DP):
        prep_w(f"in{c.o_pool + g0}", w_in_d[:, c.o_pool + g0:c.o_pool + g0 + ncol], KD, ncol)
    for g0, ncol in groups(DQK):
        prep_w(f"in{c.o_q + g0}", w_in_d[:, c.o_q + g0:c.o_q + g0 + ncol], KD, ncol)
    for g0, ncol in groups(D):
        prep_w(f"upp{g0}", w_up_pool_d[:, g0:g0 + ncol], DP // 128, ncol)
        prep_w(f"in{c.o_gp + g0}", w_in_d[:, c.o_gp + g0:c.o_gp + g0 + ncol], KD, ncol)
    for g0, ncol in groups(DQK):
        prep_w(f"in{c.o_z + g0}", w_in_d[:, c.o_z + g0:c.o_z + g0 + ncol], KD, ncol)
    for g0, ncol in groups(D):
        prep_w(f"upd{g0}", w_up_dn_d[:, g0:g0 + ncol], DQK // 128, ncol)
        prep_w(f"in{c.o_gd + g0}", w_in_d[:, c.o_gd + g0:c.o_gd + g0 + ncol], KD, ncol)
    for g0, ncol in groups(D):
        prep_w(f"out{g0}", w_out_d[:, g0:g0 + ncol], KD, ncol)
    for g0, ncol in groups(D):
        prep_w(f"pg{g0}", w_pg_d[:, g0:g0 + ncol], KD, ncol)
    for ex in range(NE):
        prep_w(f"eg{ex}", w_gate_d[ex], KD, c.DE)
        prep_w(f"eu{ex}", w_upe_d[ex], KD, c.DE)
        prep_w(f"ed{ex}", w_down_d[ex], c.DE // 128, D)

    S = [sb(f"S{g}", [128, HG, 128]) for g in range(NHG)]
    for g in range(NHG):
        pg.op("dve", lambda e, g=g: e.memset(S[g][:, :, :], 0.0), writes=[S[g]])
    NQ = 3 * DQK // 128
    halo = sb("halo", [128, NQ, 3])
    pg.op("dve", lambda e: e.memset(halo[:, :, :], 0.0), writes=[halo])
    u_prev = sb("u_prev", [128, DP], BF16)
    cnt_bc = sb("cnt_bc", [128, NE])
    pg.op("dve", lambda e: e.memset(cnt_bc[:, :], 0.0), writes=[cnt_bc])
    NST = TOK // 128
    slot_i = sb("slot_i", [128, NST, 2], I32)
    wts = sb("wts", [128, NST, 2])

    phase_mark = aoff[0]
    xt = [sb(f"xt{i}", [128, D]) for i in range(2)]
    xs = [n2b] * 2
    mT = sb_b("mT", [128, KD, T], BF16)
    raw = [sb(f"raw{i}", [128, T + 3]) for i in range(2)]
    cacc = [sb(f"cacc{i}", [128, T]) for i in range(2)]
    QKV32 = max(3 * H * T // 2, NSUB * (DP // 2) + (DP * T // 256), D)
    qkv = sb_b("qkv", [128, QKV32])
    qc = view(qkv, 0, "qc", [128, H, T], BF16)
    kc = view(qkv, H * T // 2, "kc", [128, H, T], BF16)
    vc = view(qkv, H * T, "vc", [128, H, T], BF16)
    u_tm = [view(qkv, i * (DP // 2), f"u_tm{i}", [128, DP], BF16) for i in range(NSUB)]
    dT = view(qkv, NSUB * (DP // 2), "dT", [128, DP // 128, T], BF16)
    ypT = sb("ypT", [128, DP // 128, T], BF16)
    sgd = sb("sgd", [128, KD, T], BF16)
    assert NSUB * (DP // 2) + (DP * T // 256) <= QKV32
    NCH = T // 64
    z_tm = [sb(f"z_tm{i}", [64, DQK], BF16) for i in range(NCH)]
    ba = sb("ba", [64, 2 * H])
    beta = sb("beta", [64, H]); gg = sb("gg", [64, H])
    k_tm = sb("k_tm", [64, H, 128]); k_n = sb("k_n", [64, H, 128], BF16); v_tm = sb("v_tm", [64, H, 128], BF16)
    k_nT = sb("k_nT", [128, H, 64], BF16)
    sq = sb("sq", [64, H, 128]); sqT = sb("sqT", [128, H, 64])
    ssk = sb("ssk", [64, H]); rk = sb("rk", [64, H]); rq = sb("rq", [64, H])
    o_tm = sb("o_tm", [64, H, 128])
    ydn = sb("ydn", [64, H, 128], BF16)
    ydnT = sb("ydnT", [128, H, T], BF16)
    class GB:
        pass
    GBs = []
    for g in range(NHG):
        o = GB()
        o.A_all = sb(f"A_all{g}", [64, HG, 64]); o.decT = sb(f"decT{g}", [64, HG, 64]); o.attnT = sb(f"attnT{g}", [64, HG, 64], BF16)
        o.M2 = sb(f"M2{g}", [64, HG, 64], BF16); o.Pm = [sb(f"Pm{g}{i}", [64, HG, 64], BF16) for i in range(2)]
        o.Qm = [sb(f"Qm{g}{i}", [64, HG, 64], BF16) for i in range(2)]
        o.XT = sb(f"XT{g}", [64, HG, 64], BF16)
        o.eg = sb(f"eg{g}", [64, 2 * HG]); o.egt = sb(f"egt{g}", [128, HG])
        o.kg = sb(f"kg{g}", [64, HG, 128], BF16); o.kdec = sb(f"kdec{g}", [64, HG, 128], BF16)
        o.up_sb = sb(f"up_sb{g}", [64, HG, 128]); o.wT = sb(f"wT{g}", [128, HG, 64], BF16)
        o.vnew = sb(f"vnew{g}", [64, HG, 128], BF16); o.dtmp = sb(f"dtmp{g}", [64, HG, 128])
        o.o1 = sb(f"o1{g}", [64, HG, 128]); o.sc1 = sb(f"sc1{g}", [64, HG])
        o.Sb = sb(f"Sb{g}", [128, HG, 128], BF16)
        pg.op("dve", lambda e, o=o: e.memset(o.Sb[:, :, :], 0.0), writes=[o.Sb])
        GBs.append(o)
    n2T = view(mT, 0, "n2T", [128, KD, 128])
    n2 = view(qkv, 0, "n2", [128, D])
    assert D <= QKV32 and KD * 128 <= KD * T // 2
    lg = sb("lg", [128, c.NGR + NE]); rt = sb("rt", [128, 64]); oh = sb("oh", [128, 2, NE]); ohs = sb("ohs", [128, NE], BF16)
    posf = sb("posf", [128, NE]); tmpe = sb("tmpe", [128, NE]); le = sb("le", [128, c.EPG]); ohg = sb("ohg", [128, c.NGR])
    oh1 = sb("oh1", [128, c.EPG]); oh2 = sb("oh2", [128, c.EPG]); slf = sb("slf", [128, 2])

    w_in_T = lambda c0, n: w_in_d[:, c0:c0 + n]

    def rms_rstd(src_tile, src_ap, dst, junk):
        pg.op("act", lambda e: e.activation(junk[:, :], src_ap, AF.Square, accum_out=dst), reads=[src_tile], writes=[junk, st1])
        pg.op("act", lambda e: e.activation(dst, dst, AF.Ln, scale=1.0 / D, bias=EPS), reads=[st1], writes=[st1])
        pg.op("act", lambda e: e.activation(dst, dst, AF.Exp, scale=-0.5), reads=[st1], writes=[st1])

    def make_n1T(xsrc_d, row0, s):
        xb = xt[s % 2]; xsb = xs[s % 2]
        pg.dma("sp", lambda e: e.dma_start(out=xb[:, :], in_=xsrc_d[row0:row0 + 128, :]), writes=[xb])
        rms_rstd(xb, xb[:, :], st1[:, 0:1], xsb)
        pg.op("act", lambda e: e.activation(xsb[:, :], xb[:, :], AF.Copy, scale=st1[:, 0:1]), reads=[xb, st1], writes=[xsb])
        for c0 in range(0, KD, 8):
            nb = min(8, KD - c0)
            ps = PS()
            psv = ps.ap[:, :].bitcast(BF16)

            def tr(e, c0=c0, nb=nb, psv=psv, xsb=xsb):
                for j in range(nb):
                    r = e.transpose(psv[:, j * 128:(j + 1) * 128], xsb[:, (c0 + j) * 128:(c0 + j + 1) * 128], identb[:, :])
                return r
            pg.op("pe", tr, reads=[xsb, identb], writes=[ps])
            pg.op("dve", lambda e, c0=c0, nb=nb, psv=psv, s=s: e.tensor_tensor(
                n1T[:, c0:c0 + nb, s * 128:(s + 1) * 128],
                psv[:, 0:nb * 128].rearrange("p (c t) -> p c t", c=nb),
                bc(gmixT[:, c0:c0 + nb].unsqueeze(2), [128, nb, 128]), op=ALU.mult),
                reads=[ps, gmixT], writes=[n1T])

    def mm_fm(ps, wt, wview, col0, rhs_tile, rhs_fn, kch, n):
        def f(e):
            for cc in range(kch):
                r = e.matmul(ps.ap[:, 0:n], wview[:, cc, col0:col0 + 128], rhs_fn(cc), start=(cc == 0), stop=(cc == kch - 1))
            return r
        pg.op("pe", f, reads=[wt, rhs_tile], writes=[ps])

    def mm_tm(ps, lhs_tile, lhs_fn, wt, wview, kch, ncols):
        def f(e):
            for cc in range(kch):
                r = e.matmul(ps.ap[:, 0:ncols], lhs_fn(cc), wview[:, cc, 0:ncols], start=(cc == 0), stop=(cc == kch - 1))
            return r
        pg.op("pe", f, reads=[wt, lhs_tile], writes=[ps])

    def conv_tile(ti, dst_tile, dst_ap, ps, n):
        rb = raw[ti % 2]; ca = cacc[ti % 2]
        pg.op("act", lambda e: e.activation(rb[:, 3:3 + n], ps.ap[:, 0:n], AF.Copy), reads=[ps], writes=[rb])
        pg.op("dve", lambda e: e.tensor_copy(rb[:, 0:3], halo[:, ti, :]), reads=[halo], writes=[rb])
        pg.op("dve", lambda e: e.tensor_copy(halo[:, ti, :], rb[:, n:n + 3]), reads=[rb], writes=[halo])
        pg.op("dve", lambda e: e.tensor_scalar(ca[:, 0:n], rb[:, 0:n], convT[:, 0, ti:ti + 1], None, op0=ALU.mult), reads=[rb, convT], writes=[ca])
        for k in range(1, 4):
            pg.op("dve", lambda e, k=k: e.scalar_tensor_tensor(ca[:, 0:n], rb[:, k:k + n], convT[:, k, ti:ti + 1], ca[:, 0:n],
                                                              op0=ALU.mult, op1=ALU.add), reads=[rb, convT, ca], writes=[ca])
        pg.op("act", lambda e: e.activation(dst_ap, ca[:, 0:n], AF.Silu), reads=[ca], writes=[dst_tile])

    def proj_qkv(which, n):
        dst = (qc, kc, vc)[which]
        off = (c.o_q, c.o_k, c.o_v)[which]
        for g0 in range(0, DQK, 512):
            ncol = min(512, DQK - g0)
            wt, wv = load_w(f"in{off + g0}", w_in_T(off + g0, ncol), KD, ncol)
            for j in range(ncol // 128):
                ps = PS()
                mm_fm(ps, wt, wv, j * 128, n1T, lambda cc: n1T[:, cc, 0:n], KD, n)
                hh = (g0 + j * 128) // 128
                conv_tile(which * H + hh, dst, dst[:, hh, 0:n], ps, n)

    def psb16(ps):
        return ps.ap[:, :].bitcast(BF16)

    def tm_from_fm(src, dst, tok0):
        for g in range(NHG):
            ps = PS()
            pv = psb16(ps)

            def f(e, g=g, pv=pv):
                for j in range(HG):
                    r = e.transpose(pv[0:64, j * 128:(j + 1) * 128], src[:, g * HG + j, tok0:tok0 + 64], identb[:, :])
                return r
            pg.op("pe", f, reads=[src, identb], writes=[ps])
            pg.op("act", lambda e, g=g, pv=pv: e.activation(dst[:, g * HG:(g + 1) * HG, :], pv[0:64, 0:HG * 128].rearrange("p (h d) -> p h d", h=HG), AF.Copy),
                  reads=[ps], writes=[dst])

    def beta_g(tok0):
        ps = PS()

        def f(e):
            for cc in range(KD):
                r = e.matmul(ps.ap[0:64, 0:2 * H], n1T[:, cc, tok0:tok0 + 64], wba[:, cc, :], start=(cc == 0), stop=(cc == KD - 1))
            return r
        pg.op("pe", f, reads=[n1T, wba], writes=[ps])
        pg.op("act", lambda e: e.activation(beta[:, :], ps.ap[0:64, 0:H], AF.Sigmoid), reads=[ps], writes=[beta])
        pg.op("dve", lambda e: e.tensor_tensor(ba[:, 0:H], ps.ap[0:64, H:2 * H], hp[0:64, H:2 * H], op=ALU.add), reads=[ps, hp], writes=[ba])
        pg.op("act", lambda e: e.activation(ba[:, 0:H], ba[:, 0:H], AF.Exp), reads=[ba], writes=[ba])
        pg.op("act", lambda e: e.activation(ba[:, 0:H], ba[:, 0:H], AF.Ln, bias=1.0), reads=[ba], writes=[ba])
        pg.op("dve", lambda e: e.tensor_tensor(gg[:, :], ba[:, 0:H], hp[0:64, 0:H], op=ALU.mult), reads=[ba, hp], writes=[gg])

    def r3(ap, h):
        return ap.rearrange("p (h d) -> p h d", h=h)

    def chain(g, tok0, state_only):
        B = GBs[g]
        cbanks = [0, 1, 2] if g == 0 else [3, 4, 5]

        def CPS():
            return PSP(('c', g), cbanks)
        hs = slice(g * HG, (g + 1) * HG)
        Sg = S[g]
        A_all, decT, attnT, M2, Pm, Qm, XT = B.A_all, B.decT, B.attnT, B.M2, B.Pm, B.Qm, B.XT
        eg, egt, kg, kdec, up_sb, wT, vnew, dtmp, o1, sc1, Sb = B.eg, B.egt, B.kg, B.kdec, B.up_sb, B.wT, B.vnew, B.dtmp, B.o1, B.sc1, B.Sb
        I64b = identb[0:64, 0:64]
        pg.op("dve", lambda e: e.tensor_tensor(A_all[:, :, :], bc(U[0:64, :].unsqueeze(1), [64, HG, 64]),
                                               bc(gg[:, hs].unsqueeze(2), [64, HG, 64]), op=ALU.mult), reads=[cst, gg], writes=[A_all])
        psD = CPS()

        def f(e):
            e.matmul(psD.ap[0:64, 0:HG * 64], Bm[0:64, :], A_all[:, :, :].rearrange("p h i -> p (h i)"), start=True, stop=False, skip_group_check=True)
            r = None
            for j in range(HG):
                r = e.matmul(psD.ap[0:64, j * 64:(j + 1) * 64], I64[0:64, :], NEGU[0:64, :], start=False, stop=(j == HG - 1), skip_group_check=True)
            return r
        pg.op("pe", f, reads=[A_all, cst], writes=[psD])
        psG = CPS()

        def f(e):
            e.matmul(psG.ap[0:64, 0:HG], U[0:64, :], gg[:, hs], start=True, stop=True)
            e.matmul(psG.ap[0:64, HG:2 * HG], Bm[0:64, :], gg[:, hs], start=True, stop=True)
            return e.matmul(psG.ap[:, 2 * HG:3 * HG], ONES[0:64, :], gg[:, hs], start=True, stop=True)
        pg.op("pe", f, reads=[gg, cst], writes=[psG])
        yield
        pg.op("act", lambda e: e.activation(decT[:, :, :], r3(psD.ap[0:64, 0:HG * 64], HG), AF.Exp), reads=[psD], writes=[decT])
        pg.op("act", lambda e: e.activation(eg[:, :], psG.ap[0:64, 0:2 * HG], AF.Exp), reads=[psG], writes=[eg])
        pg.op("act", lambda e: e.activation(egt[:, :], psG.ap[:, 2 * HG:3 * HG], AF.Exp), reads=[psG], writes=[egt])
        psK = CPS()

        def f(e):
            for j in range(HG):
                h = g * HG + j
                r = e.matmul(psK.ap[0:64, j * 64:(j + 1) * 64], k_nT[:, h, :], k_nT[:, h, :], start=True, stop=True)
            return r
        pg.op("pe", f, reads=[k_nT], writes=[psK])
        if not state_only:
            psQ = CPS()

            def f(e):
                for j in range(HG):
                    h = g * HG + j
                    r = e.matmul(psQ.ap[0:64, j * 64:(j + 1) * 64], k_nT[:, h, :], qc[:, h, tok0:tok0 + 64], start=True, stop=True)
                return r
            pg.op("pe", f, reads=[k_nT, qc], writes=[psQ])
        yield
        pg.op("dve", lambda e: e.tensor_tensor(o1[:, :, 0:64], r3(psK.ap[0:64, 0:HG * 64], HG), decT[:, :, :], op=ALU.mult), reads=[psK, decT], writes=[o1])
        pg.op("dve", lambda e: e.tensor_tensor(o1[:, :, 0:64], o1[:, :, 0:64], bc(SUN[0:64, :].unsqueeze(1), [64, HG, 64]), op=ALU.mult), reads=[o1, cst], writes=[o1])
        pg.op("dve", lambda e: e.tensor_tensor(M2[:, :, :], o1[:, :, 0:64], bc(beta[:, hs].unsqueeze(2), [64, HG, 64]), op=ALU.mult), reads=[o1, beta], writes=[M2])
        if not state_only:
            pg.op("dve", lambda e: e.tensor_tensor(attnT[:, :, :], r3(psQ.ap[0:64, 0:HG * 64], HG), decT[:, :, :], op=ALU.mult), reads=[psQ, decT], writes=[attnT])
        psT = CPS()
        pvT = psb16(psT)

        def f(e):
            for j in range(HG):
                r = e.transpose(pvT[0:64, j * 64:(j + 1) * 64], M2[:, j, :], I64b)
            return r
        pg.op("pe", f, reads=[M2, identb], writes=[psT])
        yield
        pg.op("act", lambda e: e.activation(Qm[0][:, :, :], r3(pvT[0:64, 0:HG * 64], HG), AF.Copy), reads=[psT], writes=[Qm[0]])
        pg.op("dve", lambda e: e.tensor_tensor(XT[:, :, :], M2[:, :, :], bc(I64[0:64, :].unsqueeze(1), [64, HG, 64]), op=ALU.add), reads=[M2, cst], writes=[XT])
        Pc, Qc = M2, Qm[0]
        for n in range(1, 6):
            Pn, Qn = Pm[n % 2], Qm[n % 2]
            last = (n == 5)
            psQ2 = CPS()

            def f(e, psQ2=psQ2, Pc=Pc, Qc=Qc):
                for j in range(HG):
                    r = e.matmul(psQ2.ap[0:64, j * 64:(j + 1) * 64], Pc[:, j, :], Qc[:, j, :], start=True, stop=True)
                return r
            pg.op("pe", f, reads=[Pc, Qc], writes=[psQ2])
            if not last:
                psP = CPS()

                def f(e, psP=psP, Pc=Pc, Qc=Qc):
                    for j in range(HG):
                        r = e.matmul(psP.ap[0:64, j * 64:(j + 1) * 64], Qc[:, j, :], Pc[:, j, :], start=True, stop=True)
                    return r
                pg.op("pe", f, reads=[Pc, Qc], writes=[psP])
            yield
            pg.op("act", lambda e, psQ2=psQ2, Qn=Qn: e.activation(Qn[:, :, :], r3(psQ2.ap[0:64, 0:HG * 64], HG), AF.Copy), reads=[psQ2], writes=[Qn])
            if not last:
                pg.op("act", lambda e, psP=psP, Pn=Pn: e.activation(Pn[:, :, :], r3(psP.ap[0:64, 0:HG * 64], HG), AF.Copy), reads=[psP], writes=[Pn])
            psX = CPS()

            def f(e, psX=psX, Qn=Qn):
                for j in range(HG):
                    r = e.matmul(psX.ap[0:64, j * 64:(j + 1) * 64], Qn[:, j, :], XT[:, j, :], start=True, stop=True)
                return r
            pg.op("pe", f, reads=[Qn, XT], writes=[psX])
            yield
            pg.op("dve", lambda e, psX=psX: e.tensor_tensor(XT[:, :, :], XT[:, :, :], r3(psX.ap[0:64, 0:HG * 64], HG), op=ALU.add), reads=[psX, XT], writes=[XT])
            Pc, Qc = Pn, Qn
        pg.op("dve", lambda e: e.tensor_tensor(kg[:, :, :], k_n[:, hs, :], bc(eg[:, 0:HG].unsqueeze(2), [64, HG, 128]), op=ALU.mult), reads=[k_n, eg], writes=[kg])
        pg.op("dve", lambda e: e.tensor_tensor(kdec[:, :, :], k_n[:, hs, :], bc(eg[:, HG:2 * HG].unsqueeze(2), [64, HG, 128]), op=ALU.mult), reads=[k_n, eg], writes=[kdec])
        psU = CPS()

        def f(e):
            for j in range(HG):
                r = e.matmul(psU.ap[0:64, j * 128:(j + 1) * 128], XT[:, j, :], v_tm[:, g * HG + j, :], start=True, stop=True)
            return r
        pg.op("pe", f, reads=[XT, v_tm], writes=[psU])
        psW = CPS()

        def f(e):
            for j in range(HG):
                r = e.matmul(psW.ap[:, j * 64:(j + 1) * 64], kg[:, j, :], XT[:, j, :], start=True, stop=True)
            return r
        pg.op("pe", f, reads=[XT, kg], writes=[psW])
        yield
        pg.op("act", lambda e: e.activation(up_sb[:, :, :], r3(psU.ap[0:64, 0:HG * 128], HG), AF.Copy), reads=[psU], writes=[up_sb])
        pg.op("act", lambda e: e.activation(wT[:, :, :], r3(psW.ap[:, 0:HG * 64], HG), AF.Copy), reads=[psW], writes=[wT])
        psWS = CPS()

        def f(e):
            for j in range(HG):
                r = e.matmul(psWS.ap[0:64, j * 128:(j + 1) * 128], wT[:, j, :], Sb[:, j, :], start=True, stop=True)
            return r
        pg.op("pe", f, reads=[wT, Sb], writes=[psWS])
        if not state_only:
            psT1 = CPS()

            def f(e):
                for j in range(HG):
                    r = e.matmul(psT1.ap[0:64, j * 128:(j + 1) * 128], qc[:, g * HG + j, tok0:tok0 + 64], Sb[:, j, :], start=True, stop=True)
                return r
            pg.op("pe", f, reads=[qc, Sb], writes=[psT1])
        yield
        pg.op("dve", lambda e: e.tensor_tensor(dtmp[:, :, :], up_sb[:, :, :], r3(psWS.ap[0:64, 0:HG * 128], HG), op=ALU.subtract), reads=[up_sb, psWS], writes=[dtmp])
        pg.op("dve", lambda e: e.tensor_tensor(vnew[:, :, :], dtmp[:, :, :], bc(beta[:, hs].unsqueeze(2), [64, HG, 128]), op=ALU.mult), reads=[dtmp, beta], writes=[vnew])
        if not state_only:
            pg.op("dve", lambda e: e.tensor_tensor(sc1[:, :], eg[:, 0:HG], rq[:, hs], op=ALU.mult), reads=[eg, rq], writes=[sc1])
            pg.op("dve", lambda e: e.tensor_tensor(o1[:, :, :], r3(psT1.ap[0:64, 0:HG * 128], HG), bc(sc1[:, :].unsqueeze(2), [64, HG, 128]), op=ALU.mult),
                  reads=[psT1, sc1], writes=[o1])
        psS = CPS()

        def f(e):
            for j in range(HG):
                r = e.matmul(psS.ap[:, j * 128:(j + 1) * 128], kdec[:, j, :], vnew[:, j, :], start=True, stop=True)
            return r
        pg.op("pe", f, reads=[kdec, vnew], writes=[psS])
        if not state_only:
            psT2 = CPS()

            def f(e):
                for j in range(HG):
                    r = e.matmul(psT2.ap[0:64, j * 128:(j + 1) * 128], attnT[:, j, :], vnew[:, j, :], start=True, stop=True)
                return r
            pg.op("pe", f, reads=[attnT, vnew], writes=[psT2])
        yield
        pg.op("dve", lambda e: e.tensor_tensor(Sg[:, :, :], Sg[:, :, :], bc(egt[:, :].unsqueeze(2), [128, HG, 128]), op=ALU.mult), reads=[Sg, egt], writes=[Sg])
        pg.op("dve", lambda e: e.tensor_tensor(Sg[:, :, :], Sg[:, :, :], r3(psS.ap[:, 0:HG * 128], HG), op=ALU.add), reads=[Sg, psS], writes=[Sg])
        pg.op("act", lambda e: e.activation(Sb[:, :, :], Sg[:, :, :], AF.Copy), reads=[Sg], writes=[Sb])
        if not state_only:
            pg.op("dve", lambda e: e.tensor_tensor(dtmp[:, :, :], r3(psT2.ap[0:64, 0:HG * 128], HG), bc(rq[:, hs].unsqueeze(2), [64, HG, 128]), op=ALU.mult),
                  reads=[psT2, rq], writes=[dtmp])
            pg.op("dve", lambda e: e.tensor_tensor(o_tm[:, hs, :], dtmp[:, :, :], o1[:, :, :], op=ALU.add), reads=[o1, dtmp], writes=[o_tm])
        yield

    def delta_chunk(ci, state_only, fill=iter(())):
        tok0 = ci * 64
        beta_g(tok0)
        tm_from_fm(kc, k_tm, tok0)
        tm_from_fm(vc, v_tm, tok0)
        pg.op("dve", lambda e: e.tensor_tensor(sq[:, :, :], k_tm[:, :, :], k_tm[:, :, :], op=ALU.mult), reads=[k_tm], writes=[sq])
        pg.op("dve", lambda e: e.tensor_reduce(ssk[:, :], sq[:, :, :], axis=AX.X, op=ALU.add), reads=[sq], writes=[ssk])
        pg.op("act", lambda e: e.activation(rk[:, :], ssk[:, :], AF.Ln, bias=EPS), reads=[ssk], writes=[rk])
        pg.op("act", lambda e: e.activation(rk[:, :], rk[:, :], AF.Exp, scale=-0.5), reads=[rk], writes=[rk])
        pg.op("dve", lambda e: e.tensor_tensor(k_n[:, :, :], k_tm[:, :, :], bc(rk[:, :].unsqueeze(2), [64, H, 128]), op=ALU.mult), reads=[k_tm, rk], writes=[k_n])
        for g in range(NHG):
            ps = PS()
            pv = psb16(ps)

            def f(e, g=g, pv=pv):
                for j in range(HG):
                    r = e.transpose(pv[:, j * 64:(j + 1) * 64], k_n[:, g * HG + j, :], identb[0:64, 0:64])
                return r
            pg.op("pe", f, reads=[k_n, identb], writes=[ps])
            pg.op("act", lambda e, g=g, pv=pv: e.activation(k_nT[:, g * HG:(g + 1) * HG, :], r3(pv[:, 0:HG * 64], HG), AF.Copy), reads=[ps], writes=[k_nT])
        if not state_only:
            pg.op("act", lambda e: e.activation(sqT[:, :, :], qc[:, :, tok0:tok0 + 64], AF.Square), reads=[qc], writes=[sqT])
            ps = PS()

            def f(e, ps=ps):
                for h in range(H):
                    r = e.matmul(ps.ap[0:64, h:h + 1], sqT[:, h, :], ONES[:, 0:1], start=True, stop=True)
                return r
            pg.op("pe", f, reads=[sqT, cst], writes=[ps])
            pg.op("act", lambda e, ps=ps: e.activation(rq[:, :], ps.ap[0:64, 0:H], AF.Ln, bias=EPS), reads=[ps], writes=[rq])
            pg.op("act", lambda e: e.activation(rq[:, :], rq[:, :], AF.Exp, scale=-0.5), reads=[rq], writes=[rq])
            pg.op("dve", lambda e: e.tensor_scalar(rq[:, :], rq[:, :], 128.0 ** -0.5, None, op0=ALU.mult), reads=[rq], writes=[rq])
        gens = [chain(g, tok0, state_only) for g in range(NHG)]
        alive = list(gens)
        while alive:
            for gen in list(alive):
                try:
                    next(gen)
                except StopIteration:
                    alive.remove(gen)
            next(fill, None)
        if not state_only:
            pg.op("dve", lambda e: e.tensor_tensor(sq[:, :, :], o_tm[:, :, :], o_tm[:, :, :], op=ALU.mult), reads=[o_tm], writes=[sq])
            pg.op("dve", lambda e: e.tensor_reduce(ssk[:, :], sq[:, :, :], axis=AX.X, op=ALU.add), reads=[sq], writes=[ssk])
            pg.op("act", lambda e: e.activation(ssk[:, :], ssk[:, :], AF.Ln, scale=1.0 / 128, bias=EPS), reads=[ssk], writes=[ssk])
            pg.op("act", lambda e: e.activation(ssk[:, :], ssk[:, :], AF.Exp, scale=-0.5), reads=[ssk], writes=[ssk])
            pg.op("dve", lambda e: e.tensor_tensor(o_tm[:, :, :], o_tm[:, :, :], bc(ssk[:, :].unsqueeze(2), [64, H, 128]), op=ALU.mult), reads=[o_tm, ssk], writes=[o_tm])
            pg.op("dve", lambda e: e.tensor_tensor(o_tm[:, :, :], o_tm[:, :, :], bc(g_dn[0:64, :].unsqueeze(1), [64, H, 128]), op=ALU.mult), reads=[o_tm, g_dn], writes=[o_tm])
            zs = z_tm[ci]
            pg.op("act", lambda e: e.activation(sq[:, :, :], r3(zs[:, :], H), AF.Silu), reads=[zs], writes=[sq])
            pg.op("dve", lambda e: e.tensor_tensor(ydn[:, :, :], o_tm[:, :, :], sq[:, :, :], op=ALU.mult), reads=[o_tm, sq], writes=[ydn])
            ps = PS()
            psv = psb16(ps)

            def f(e, psv=psv):
                for h in range(H):
                    r = e.transpose(psv[:, h * 64:(h + 1) * 64], ydn[:, h, :], identb[0:64, 0:64])
                return r
            pg.op("pe", f, reads=[ydn, identb], writes=[ps])
            pg.op("act", lambda e, psv=psv: e.activation(ydnT[:, :, tok0:tok0 + 64], r3(psv[:, 0:H * 64], H), AF.Copy), reads=[ps], writes=[ydnT])

    def router_and_scatter(hb, st):
        rms_rstd(hb, hb[:, :], st1[:, 1:2], n2b)
        pg.op("act", lambda e: e.activation(n2[:, :], hb[:, :], AF.Copy, scale=st1[:, 1:2]), reads=[hb, st1], writes=[n2])
        pg.op("act", lambda e: e.activation(n2b[:, :], n2[:, :], AF.Copy), reads=[n2], writes=[n2b])
        for c0 in range(0, KD, 4):
            nb = min(4, KD - c0)
            ps = PS()

            def f(e, c0=c0, ps=ps, nb=nb):
                for j in range(nb):
                    r = e.transpose(ps.ap[:, j * 128:(j + 1) * 128], n2[:, (c0 + j) * 128:(c0 + j + 1) * 128], ident)
                return r
            pg.op("pe", f, reads=[n2, cst], writes=[ps])
            pg.op("dve", lambda e, c0=c0, ps=ps, nb=nb: e.tensor_tensor(n2T[:, c0:c0 + nb, :], ps.ap[:, 0:nb * 128].rearrange("p (c t) -> p c t", c=nb),
                                                                     bc(gmoeT[:, c0:c0 + nb].unsqueeze(2), [128, nb, 128]), op=ALU.mult), reads=[ps, gmoeT], writes=[n2T])
        NL = c.NGR + NE
        ps = PS()

        def f(e, ps=ps):
            for cc in range(KD):
                r = e.matmul(ps.ap[:, 0:NL], n2T[:, cc, :], wr[:, cc, :], start=(cc == 0), stop=(cc == KD - 1))
            return r
        pg.op("pe", f, reads=[n2T, wr], writes=[ps])
        pg.op("dve", lambda e, ps=ps: e.tensor_tensor(lg[:, :], ps.ap[:, 0:NL], brep[:, :], op=ALU.add), reads=[ps, brep], writes=[lg])
        G = c.NGR; E = c.EPG
        pg.op("dve", lambda e: e.tensor_reduce(rt[:, 0:1], lg[:, 0:G], axis=AX.X, op=ALU.max), reads=[lg], writes=[rt])
        pg.op("dve", lambda e: e.tensor_scalar(ohg[:, :], lg[:, 0:G], rt[:, 0:1], None, op0=ALU.is_equal), reads=[lg, rt], writes=[ohg])
        pg.op("dve", lambda e: e.tensor_scalar(rt[:, 8:8 + G], lg[:, 0:G], rt[:, 0:1], None, op0=ALU.subtract), reads=[lg, rt], writes=[rt])
        pg.op("act", lambda e: e.activation(rt[:, 8:8 + G], rt[:, 8:8 + G], AF.Exp), reads=[rt], writes=[rt])
        pg.op("dve", lambda e: e.tensor_reduce(rt[:, 1:2], rt[:, 8:8 + G], axis=AX.X, op=ALU.add), reads=[rt], writes=[rt])
        pg.op("dve", lambda e: e.reciprocal(rt[:, 1:2], rt[:, 1:2]), reads=[rt], writes=[rt])
        pg.op("dve", lambda e: e.tensor_tensor(tmpe[:, :].rearrange("p (g x) -> p g x", g=G), lg[:, G:].rearrange("p (g x) -> p g x", g=G),
                                               bc(ohg[:, :].unsqueeze(2), [128, G, E]), op=ALU.mult), reads=[lg, ohg], writes=[tmpe])
        pg.op("dve", lambda e: e.tensor_reduce(le[:, :], tmpe[:, :].rearrange("p (g x) -> p x g", g=G), axis=AX.X, op=ALU.add), reads=[tmpe], writes=[le])
        pg.op("dve", lambda e: e.tensor_reduce(rt[:, 2:3], le[:, :], axis=AX.X, op=ALU.max), reads=[le], writes=[rt])
        pg.op("dve", lambda e: e.tensor_scalar(oh1[:, :], le[:, :], rt[:, 2:3], None, op0=ALU.is_equal), reads=[le, rt], writes=[oh1])
        pg.op("dve", lambda e: e.scalar_tensor_tensor(le[:, :], oh1[:, :], NEG, le[:, :], op0=ALU.mult, op1=ALU.add), reads=[oh1, le], writes=[le])
        pg.op("dve", lambda e: e.tensor_reduce(rt[:, 3:4], le[:, :], axis=AX.X, op=ALU.max), reads=[le], writes=[rt])
        pg.op("dve", lambda e: e.tensor_scalar(oh2[:, :], le[:, :], rt[:, 3:4], None, op0=ALU.is_equal), reads=[le, rt], writes=[oh2])
        pg.op("dve", lambda e: e.tensor_tensor(rt[:, 4:5], rt[:, 2:3], rt[:, 3:4], op=ALU.subtract), reads=[rt], writes=[rt])
        pg.op("act", lambda e: e.activation(rt[:, 4:5], rt[:, 4:5], AF.Sigmoid), reads=[rt], writes=[rt])
        pg.op("dve", lambda e: e.tensor_tensor(wts[:, st, 0:1], rt[:, 4:5], rt[:, 1:2], op=ALU.mult), reads=[rt], writes=[wts])
        pg.op("dve", lambda e: e.tensor_tensor(wts[:, st, 1:2], rt[:, 1:2], wts[:, st, 0:1], op=ALU.subtract), reads=[rt, wts], writes=[wts])
        for k, ohk in enumerate((oh1, oh2)):
            pg.op("dve", lambda e, k=k, ohk=ohk: e.tensor_tensor(oh[:, k, :].rearrange("p (g x) -> p g x", g=G), bc(ohg[:, :].unsqueeze(2), [128, G, E]),
                                                                 bc(ohk[:, :].unsqueeze(1), [128, G, E]), op=ALU.mult), reads=[ohg, ohk], writes=[oh])
        pg.op("dve", lambda e: e.tensor_tensor(ohs[:, :], oh[:, 0, :], oh[:, 1, :], op=ALU.add), reads=[oh], writes=[ohs])
        ps = PS()

        def f(e, ps=ps):
            e.matmul(ps.ap[:, 0:NE], sltb[:, :], ohs[:, :], start=True, stop=True)
            return e.matmul(ps.ap[:, 64:64 + NE], onesb[:, :], ohs[:, :], start=True, stop=True)
        pg.op("pe", f, reads=[sltb, onesb, ohs], writes=[ps])
        pg.op("dve", lambda e, ps=ps: e.tensor_tensor(posf[:, :], ps.ap[:, 0:NE], cnt_bc[:, :], op=ALU.add), reads=[ps, cnt_bc], writes=[posf])
        pg.op("dve", lambda e: e.tensor_tensor(posf[:, :], posf[:, :], IOTA_E[:, 0:NE], op=ALU.add), reads=[posf, cst], writes=[posf])
        pg.op("dve", lambda e, ps=ps: e.tensor_tensor(cnt_bc[:, :], cnt_bc[:, :], ps.ap[:, 64:64 + NE], op=ALU.add), reads=[ps, cnt_bc], writes=[cnt_bc])
        for k in range(2):
            pg.op("dve", lambda e, k=k: e.tensor_tensor(tmpe[:, :], posf[:, :], oh[:, k, :], op=ALU.mult), reads=[posf, oh], writes=[tmpe])
            pg.op("dve", lambda e, k=k: e.tensor_reduce(slf[:, k:k + 1], tmpe[:, :], axis=AX.X, op=ALU.add), reads=[tmpe], writes=[slf])
        pg.op("dve", lambda e: e.tensor_copy(slot_i[:, st, :], slf[:, :]), reads=[slf], writes=[slot_i])
        for k in range(2):
            pg.dma("pool", lambda e, k=k: e.indirect_dma_start(out=xe_d, out_offset=bass.IndirectOffsetOnAxis(ap=slot_i[:, st, k:k + 1], axis=0),
                                                               in_=n2b[:, :], in_offset=None), reads=[n2b, slot_i], writes=[])

    def block(xsrc_d, blk, state_only, warm=False):
        n = T
        for s in range(NSUB):
            make_n1T(xsrc_d, blk * T + s * 128, s)
        if not state_only:
            for g0 in range(0, DP, 512):
                ncol = min(512, DP - g0)
                wt, wv = load_w(f"in{c.o_pool + g0}", w_in_T(c.o_pool + g0, ncol), KD, ncol)
                for s in range(NSUB):
                    ps = PS()
                    mm_tm(ps, n1T, lambda cc, s=s: n1T[:, cc, s * 128:(s + 1) * 128], wt, wv, KD, ncol)
                    pg.op("act", lambda e, ps=ps, s=s, g0=g0, ncol=ncol: e.activation(u_tm[s][:, g0:g0 + ncol], ps.ap[:, 0:ncol], AF.Copy), reads=[ps], writes=[u_tm[s]])
            for ct in range(DP // 128):
                g = ct // GK
                for s in range(NSUB):
                    ps = PS()
                    prev = u_prev if s == 0 else u_tm[s - 1]
                    first = (blk == 0 and s == 0)

                    def f(e, ps=ps, prev=prev, s=s, g=g, ct=ct, first=first):
                        e.matmul(ps.ap[:, 0:128], prev[:, ct * 128:(ct + 1) * 128], bandb[:, g, 0, :], start=True, stop=False)
                        return e.matmul(ps.ap[:, 0:128], u_tm[s][:, ct * 128:(ct + 1) * 128], bandb[:, g, 2 if first else 1, :], start=False, stop=True)
                    pg.op("pe", f, reads=[prev, u_tm[s], bandb], writes=[ps])
                    pg.op("act", lambda e, ps=ps, s=s, ct=ct: e.activation(dT[:, ct, s * 128:(s + 1) * 128], ps.ap[:, 0:128], AF.Copy), reads=[ps], writes=[dT])
            pg.op("dve", lambda e: e.tensor_copy(u_prev[:, :], u_tm[NSUB - 1][:, :]), reads=[u_tm[NSUB - 1]], writes=[u_prev])
            for g in range(4):
                for oc in range(GK):
                    ps = PS()

                    def f(e, ps=ps, g=g, oc=oc):
                        for kk in range(GK):
                            r = e.matmul(ps.ap[:, 0:n], wgrp[:, g, kk, oc * 128:(oc + 1) * 128], dT[:, g * GK + kk, :], start=(kk == 0), stop=(kk == GK - 1))
                        return r
                    pg.op("pe", f, reads=[wgrp, dT], writes=[ps])
                    ot = g * GK + oc
                    pg.op("act", lambda e, ps=ps, ot=ot: e.activation(ypT[:, ot, :], ps.ap[:, 0:n], AF.Copy, scale=pscaleT[:, ot:ot + 1]), reads=[ps, pscaleT], writes=[ypT])
            for g0 in range(0, DQK, 512):
                ncol = min(512, DQK - g0)
                wt, wv = load_w(f"in{c.o_z + g0}", w_in_T(c.o_z + g0, ncol), KD, ncol)
                for ci in range(NCH):
                    ps = PS()

                    def f(e, ps=ps, ci=ci, wv=wv, ncol=ncol):
                        for cc in range(KD):
                            r = e.matmul(ps.ap[0:64, 0:ncol], n1T[:, cc, ci * 64:(ci + 1) * 64], wv[:, cc, 0:ncol], start=(cc == 0), stop=(cc == KD - 1))
                        return r
                    pg.op("pe", f, reads=[wt, n1T], writes=[ps])
                    pg.op("act", lambda e, ps=ps, ci=ci, g0=g0, ncol=ncol: e.activation(z_tm[ci][:, g0:g0 + ncol], ps.ap[0:64, 0:ncol], AF.Copy), reads=[ps], writes=[z_tm[ci]])
            proj_qkv(0, n)
        proj_qkv(1, n)
        proj_qkv(2, n)
        def filler():
            for g0 in range(0, D, 512):
                ncol = min(512, D - g0)
                wtu, wvu = load_w(f"upp{g0}", w_up_pool_d[:, g0:g0 + ncol], DP // 128, ncol)
                wtg, wvg = load_w(f"in{c.o_gp + g0}", w_in_T(c.o_gp + g0, ncol), KD, ncol)
                for j in range(ncol // 128):
                    psa = PSP('f', [6, 7]); psg = PSP('f', [6, 7])
                    mm_fm(psa, wtu, wvu, j * 128, ypT, lambda cc: ypT[:, cc, :], DP // 128, n)
                    mm_fm(psg, wtg, wvg, j * 128, n1T, lambda cc: n1T[:, cc, :], KD, n)
                    pg.op("act", lambda e, psg=psg: e.activation(sg[:, 0:n], psg.ap[:, 0:n], AF.Sigmoid), reads=[psg], writes=[sg])
                    ot = (g0 + j * 128) // 128
                    pg.op("dve", lambda e, psa=psa, ot=ot: e.tensor_tensor(mT[:, ot, :], psa.ap[:, 0:n], sg[:, 0:n], op=ALU.mult), reads=[psa, sg], writes=[mT])
                    yield
            for g0 in range(0, D, 512):
                ncol = min(512, D - g0)
                wtg, wvg = load_w(f"in{c.o_gd + g0}", w_in_T(c.o_gd + g0, ncol), KD, ncol)
                for j in range(ncol // 128):
                    psg = PSP('f', [6, 7])
                    mm_fm(psg, wtg, wvg, j * 128, n1T, lambda cc: n1T[:, cc, :], KD, n)
                    ot = (g0 + j * 128) // 128
                    pg.op("act", lambda e, psg=psg, ot=ot: e.activation(sgd[:, ot, :], psg.ap[:, 0:n], AF.Sigmoid), reads=[psg], writes=[sgd])
                    yield

        fill = iter(()) if state_only else filler()
        for ci in range(NCH):
            delta_chunk(ci, state_only, fill)
        for _ in fill:
            pass
        if state_only:
            return
        for g0 in range(0, D, 512):
            ncol = min(512, D - g0)
            wtu, wvu = load_w(f"upd{g0}", w_up_dn_d[:, g0:g0 + ncol], DQK // 128, ncol)
            for j in range(ncol // 128):
                psa = PS()
                mm_fm(psa, wtu, wvu, j * 128, ydnT, lambda cc: ydnT[:, cc, :], DQK // 128, n)
                ot = (g0 + j * 128) // 128
                pg.op("dve", lambda e, psa=psa, ot=ot: e.tensor_tensor(sg[:, 0:n], psa.ap[:, 0:n], sgd[:, ot, :], op=ALU.mult), reads=[psa, sgd], writes=[sg])
                pg.op("dve", lambda e, ot=ot: e.tensor_tensor(mT[:, ot, :], mT[:, ot, :], sg[:, 0:n], op=ALU.add), reads=[mT, sg], writes=[mT])
        assert NSUB <= 2
        for s in range(NSUB):
            hb = xt[s % 2]
            row0 = blk * T + s * 128
            pg.dma("sp", lambda e, hb=hb, row0=row0: e.dma_start(out=hb[:, :], in_=xsrc_d[row0:row0 + 128, :]), writes=[hb])
        for g0 in range(0, D, 512):
            ncol = min(512, D - g0)
            wt, wv = load_w(f"out{g0}", w_out_d[:, g0:g0 + ncol], KD, ncol)
            for s in range(NSUB):
                hb = xt[s % 2]
                ps = PS()
                mm_tm(ps, mT, lambda cc, s=s: mT[:, cc, s * 128:(s + 1) * 128], wt, wv, KD, ncol)
                pg.op("dve", lambda e, ps=ps, hb=hb, g0=g0, ncol=ncol: e.tensor_tensor(hb[:, g0:g0 + ncol], hb[:, g0:g0 + ncol], ps.ap[:, 0:ncol], op=ALU.add),
                      reads=[ps, hb], writes=[hb])
        for s in range(NSUB):
            hb = xt[s % 2]
            row0 = blk * T + s * 128
            pg.dma("sp", lambda e, hb=hb, row0=row0: e.dma_start(out=h1s_d[row0:row0 + 128, :], in_=hb[:, :]), reads=[hb], writes=[])
            router_and_scatter(hb, blk * NSUB + s)

    if prefix:
        for blk in range(NBLK):
            block(xp_d, blk, True)
    make_n1T(xw_d, 0, 0)
    for g0 in range(0, DP, 512):
        ncol = min(512, DP - g0)
        wt, wv = load_w(f"in{c.o_pool + g0}", w_in_T(c.o_pool + g0, ncol), KD, ncol)
        ps = PS()
        mm_tm(ps, n1T, lambda cc: n1T[:, cc, 0:128], wt, wv, KD, ncol)
        pg.op("act", lambda e, ps=ps, g0=g0, ncol=ncol: e.activation(u_prev[:, g0:g0 + ncol], ps.ap[:, 0:ncol], AF.Copy), reads=[ps], writes=[u_prev])
    for g0 in range(0, DQK, 512):
        ncol = min(512, DQK - g0)
        wt, wv = load_w(f"in{c.o_q + g0}", w_in_T(c.o_q + g0, ncol), KD, ncol)
        for j in range(ncol // 128):
            ps = PS()
            mm_fm(ps, wt, wv, j * 128, n1T, lambda cc: n1T[:, cc, 0:128], KD, 128)
            hh = (g0 + j * 128) // 128
            pg.op("act", lambda e, ps=ps, hh=hh: e.activation(halo[:, hh, :], ps.ap[:, 125:128], AF.Copy), reads=[ps], writes=[halo])
    if not prefix:
        pass
    pg.wait_tokens("pool", zero_toks)
    for blk in range(NBLK):
        block(x_d, blk, False)

    pg.barrier()
    aoff[0] = phase_mark
    xe_sb = sb("xe_sb", [128, NCAPT, D], BF16)
    xeT = sb("xeT", [128, KD, CAP], BF16)
    hmT = sb("hmT", [128, c.DE // 128, CAP], BF16)
    sgm = sb("sgm", [128, CAP])
    yb = [sb(f"yb{i}", [128, D]) for i in range(2)]
    for ex in range(NE):
        pg.dma("sp", lambda e, ex=ex: e.dma_start(out=xe_sb[:, :, :], in_=xe_d[ex * CAP:(ex + 1) * CAP, :].rearrange("(t p) d -> p t d", p=128)), writes=[xe_sb])
        for t in range(NCAPT):
            for c0 in range(0, KD, 8):
                nb = min(8, KD - c0)
                ps = PS()
                psv = ps.ap[:, :].bitcast(BF16)

                def f(e, t=t, c0=c0, nb=nb, psv=psv):
                    for j in range(nb):
                        r = e.transpose(psv[:, j * 128:(j + 1) * 128], xe_sb[:, t, (c0 + j) * 128:(c0 + j + 1) * 128], identb[:, :])
                    return r
                pg.op("pe", f, reads=[xe_sb, identb], writes=[ps])
                pg.op("dve", lambda e, t=t, c0=c0, nb=nb, psv=psv: e.tensor_tensor(xeT[:, c0:c0 + nb, t * 128:(t + 1) * 128], psv[:, 0:nb * 128].rearrange("p (c t) -> p c t", c=nb),
                                                                                 bc(gmoeT[:, c0:c0 + nb].unsqueeze(2), [128, nb, 128]), op=ALU.mult), reads=[ps, gmoeT], writes=[xeT])
        wtg, wvg = load_w(f"eg{ex}", w_gate_d[ex], KD, c.DE)
        wtu, wvu = load_w(f"eu{ex}", w_upe_d[ex], KD, c.DE)
        for ft in range(c.DE // 128):
            psg = PS(); psu = PS()
            mm_fm(psg, wtg, wvg, ft * 128, xeT, lambda cc: xeT[:, cc, :], KD, CAP)
            mm_fm(psu, wtu, wvu, ft * 128, xeT, lambda cc: xeT[:, cc, :], KD, CAP)
            pg.op("act", lambda e, psg=psg: e.activation(sgm[:, :], psg.ap[:, 0:CAP], AF.Silu), reads=[psg], writes=[sgm])
            pg.op("dve", lambda e, psu=psu, ft=ft: e.tensor_tensor(hmT[:, ft, :], psu.ap[:, 0:CAP], sgm[:, :], op=ALU.mult), reads=[psu, sgm], writes=[hmT])
        FK = c.DE // 128
        t_d, wvd = load_w(f"ed{ex}", w_down_d[ex], FK, D)
        for t in range(NCAPT):
            ybt = yb[t % 2]
            for g0 in range(0, D, 512):
                ncol = min(512, D - g0)
                ps = PS()

                def f(e, ps=ps, t=t, g0=g0, ncol=ncol, wvd=wvd):
                    for kk in range(FK):
                        r = e.matmul(ps.ap[:, 0:ncol], hmT[:, kk, t * 128:(t + 1) * 128], wvd[:, kk, g0:g0 + ncol], start=(kk == 0), stop=(kk == FK - 1))
                    return r
                pg.op("pe", f, reads=[hmT, t_d], writes=[ps])
                pg.op("act", lambda e, ps=ps, ybt=ybt, g0=g0, ncol=ncol: e.activation(ybt[:, g0:g0 + ncol], ps.ap[:, 0:ncol], AF.Copy), reads=[ps], writes=[ybt])
            r0 = ex * CAP + t * 128
            pg.dma("sp", lambda e, ybt=ybt, r0=r0: e.dma_start(out=ye_d[r0:r0 + 128, :], in_=ybt[:, :]), reads=[ybt], writes=[])

    pg.barrier()
    aoff[0] = phase_mark
    wpp = sb("wpp", [128, c.DPLE // 128, D], BF16)
    pg.dma("pool", lambda e: e.dma_start(out=wpp[:, :, :], in_=w_pp_d.rearrange("(c p) n -> p c n", p=128)), writes=[wpp])
    g_fin = load_rep("g_fin", norm_final_d, D)
    y1 = [sb(f"y1_{i}", [128, D]) for i in range(2)]
    y2 = [sb(f"y2_{i}", [128, D]) for i in range(2)]
    h2 = [sb(f"h2_{i}", [128, D]) for i in range(NSUB)]
    pt = sb("pt", [128, c.DPLE]); ptb = sb("ptb", [128, c.DPLE], BF16)
    pT = sb("pT", [128, c.DPLE // 128, T], BF16)
    PK = c.DPLE // 128
    for blk in range(NBLK):
        for s in range(NSUB):
            st = blk * NSUB + s
            row0 = st * 128
            hb = h2[s]
            pg.dma("sp", lambda e, hb=hb, row0=row0: e.dma_start(out=hb[:, :], in_=h1s_d[row0:row0 + 128, :]), writes=[hb])
            ya, yb2 = y1[st % 2], y2[st % 2]
            pg.dma("pool", lambda e, ya=ya, st=st: e.indirect_dma_start(out=ya[:, :], out_offset=None, in_=ye_d,
                                                                        in_offset=bass.IndirectOffsetOnAxis(ap=slot_i[:, st, 0:1], axis=0)), reads=[slot_i], writes=[ya])
            pg.dma("pool", lambda e, yb2=yb2, st=st: e.indirect_dma_start(out=yb2[:, :], out_offset=None, in_=ye_d,
                                                                          in_offset=bass.IndirectOffsetOnAxis(ap=slot_i[:, st, 1:2], axis=0)), reads=[slot_i], writes=[yb2])
            pg.op("dve", lambda e, hb=hb, ya=ya, st=st: e.scalar_tensor_tensor(hb[:, :], ya[:, :], wts[:, st, 0:1], hb[:, :], op0=ALU.mult, op1=ALU.add), reads=[ya, wts, hb], writes=[hb])
            pg.op("dve", lambda e, hb=hb, yb2=yb2, st=st: e.scalar_tensor_tensor(hb[:, :], yb2[:, :], wts[:, st, 1:2], hb[:, :], op0=ALU.mult, op1=ALU.add), reads=[yb2, wts, hb], writes=[hb])
            rms_rstd(hb, hb[:, :], st1[:, 2:3], n2b)
            pg.op("act", lambda e, hb=hb: e.activation(n2b[:, :], hb[:, :], AF.Copy, scale=st1[:, 2:3]), reads=[hb, st1], writes=[n2b])
            for c0 in range(0, KD, 8):
                nb = min(8, KD - c0)
                ps = PS()
                psv = ps.ap[:, :].bitcast(BF16)

                def f(e, c0=c0, nb=nb, psv=psv):
                    for j in range(nb):
                        r = e.transpose(psv[:, j * 128:(j + 1) * 128], n2b[:, (c0 + j) * 128:(c0 + j + 1) * 128], identb[:, :])
                    return r
                pg.op("pe", f, reads=[n2b, identb], writes=[ps])
                pg.op("dve", lambda e, c0=c0, nb=nb, psv=psv, s=s: e.tensor_tensor(n1T[:, c0:c0 + nb, s * 128:(s + 1) * 128], psv[:, 0:nb * 128].rearrange("p (c t) -> p c t", c=nb),
                                                                                 bc(gpleT[:, c0:c0 + nb].unsqueeze(2), [128, nb, 128]), op=ALU.mult), reads=[ps, gpleT], writes=[n1T])
            pg.dma("sp", lambda e, row0=row0: e.dma_start(out=pt[:, :], in_=p_d[row0:row0 + 128, :]), writes=[pt])
            pg.op("act", lambda e: e.activation(ptb[:, :], pt[:, :], AF.Copy), reads=[pt], writes=[ptb])
            ps = PS()
            psv = ps.ap[:, :].bitcast(BF16)

            def f(e, psv=psv):
                for j in range(PK):
                    r = e.transpose(psv[:, j * 128:(j + 1) * 128], ptb[:, j * 128:(j + 1) * 128], identb[:, :])
                return r
            pg.op("pe", f, reads=[ptb, identb], writes=[ps])
            pg.op("act", lambda e, psv=psv, s=s: e.activation(pT[:, :, s * 128:(s + 1) * 128], psv[:, 0:PK * 128].rearrange("p (c t) -> p c t", c=PK), AF.Copy), reads=[ps], writes=[pT])
        for g0 in range(0, D, 512):
            ncol = min(512, D - g0)
            wt, wv = load_w(f"pg{g0}", w_pg_d[:, g0:g0 + ncol], KD, ncol)
            for s in range(NSUB):
                hb = h2[s]
                psg = PS(); psp = PS()
                mm_tm(psg, n1T, lambda cc, s=s: n1T[:, cc, s * 128:(s + 1) * 128], wt, wv, KD, ncol)

                def f(e, psp=psp, s=s, g0=g0, ncol=ncol):
                    for kk in range(PK):
                        r = e.matmul(psp.ap[:, 0:ncol], pT[:, kk, s * 128:(s + 1) * 128], wpp[:, kk, g0:g0 + ncol], start=(kk == 0), stop=(kk == PK - 1))
                    return r
                pg.op("pe", f, reads=[pT, wpp], writes=[psp])
                pg.op("act", lambda e, psg=psg, ncol=ncol: e.activation(sg[:, 0:ncol], psg.ap[:, 0:ncol], AF.Sigmoid), reads=[psg], writes=[sg])
                pg.op("dve", lambda e, psp=psp, ncol=ncol: e.tensor_tensor(sg[:, 0:ncol], sg[:, 0:ncol], psp.ap[:, 0:ncol], op=ALU.mult), reads=[psp, sg], writes=[sg])
                pg.op("dve", lambda e, hb=hb, g0=g0, ncol=ncol: e.tensor_tensor(hb[:, g0:g0 + ncol], hb[:, g0:g0 + ncol], sg[:, 0:ncol], op=ALU.add), reads=[hb, sg], writes=[hb])
        for s in range(NSUB):
            hb = h2[s]
            row0 = (blk * NSUB + s) * 128
            rms_rstd(hb, hb[:, :], st1[:, 3:4], n2b)
            pg.op("dve", lambda e, hb=hb: e.scalar_tensor_tensor(hb[:, :], hb[:, :], st1[:, 3:4], g_fin[:, :], op0=ALU.mult, op1=ALU.mult), reads=[hb, st1, g_fin], writes=[hb])
            pg.dma("sp", lambda e, hb=hb, row0=row0: e.dma_start(out=out_d[row0:row0 + 128, :], in_=hb[:, :]), reads=[hb], writes=[])

    build.stats = dict(arena_cols=amax[0], nsem=pg.nsem, cnt=dict(pg.cnt), lens={e: len(pg.lists[e]) for e in pg.ENGS})
    pg.finalize()
    es.close()
    return nc


def make_consts(cfg, first_half):
    cst = np.zeros((128, 1024), np.float32)
    cst[:, 0:128] = np.eye(128)
    m = np.arange(128)[:, None] % 64
    i = np.arange(64)[None, :]
    cst[:, 128:192] = (m <= i)
    cst[:, 192:256] = (m > i)
    cst[:, 256:320] = NEG * (m > i)
    cst[:, 320:384] = -1.0 * (m < i)
    cst[:, 384:448] = (m == i)
    cst[:, 448:576] = 1.0
    tp = np.arange(128)[:, None]
    t = np.arange(128)[None, :]
    cst[:, 576:704] = (tp < t)
    cst[:, 704:704 + cfg.NE] = (np.arange(cfg.NE) * cfg.CAP)[None, :]
    band = np.zeros((4, 3, 128, 128), np.float32)
    for g, w in enumerate(cfg.WINS):
        cur = ((tp <= t) & (tp > t - w)).astype(np.float32) / w - (tp == t)
        prev = ((tp - 128 <= t) & (tp - 128 > t - w)).astype(np.float32) / w
        cnt = np.minimum(t + 1, w).astype(np.float32)
        fst = ((tp <= t) & (tp > t - w)).astype(np.float32) / cnt - (tp == t)
        band[g, 0] = prev
        band[g, 1] = cur
        band[g, 2] = fst if first_half else cur
    return cst, band


_CACHE = {}


def kernel(**inputs):
    cfg = Cfg()
    x = np.asarray(inputs["x"])
    B, SEQ, D = x.shape
    ncores = 8
    half = SEQ // 2
    names = ["norm_mix", "w_in", "pool_w", "pool_scale", "conv_w", "a_log", "dt_bias", "dn_norm", "w_up_pool", "w_up_dn",
             "w_out", "norm_moe", "w_router_group", "b_router_group", "w_router_expert", "b_router_expert", "w_gate", "w_up",
             "w_down", "norm_ple", "w_ple_gate", "w_ple_proj"]
    shared = {n: np.ascontiguousarray(np.asarray(inputs[n])[0], dtype=np.float32) for n in names}
    shared["norm_final"] = np.ascontiguousarray(inputs["norm_final"], dtype=np.float32)
    p = np.asarray(inputs["p"])[0]
    in_maps = []
    zeros_pre = np.zeros((half, D), np.float32)
    for cidx in range(ncores):
        b, hf = divmod(cidx, 2)
        m = dict(shared)
        m["x"] = np.ascontiguousarray(x[b, hf * half:(hf + 1) * half])
        m["xpre"] = zeros_pre if hf == 0 else np.ascontiguousarray(x[b, 0:half])
        m["xwarm"] = np.zeros((128, D), np.float32) if hf == 0 else np.ascontiguousarray(x[b, half - 128:half])
        m["p"] = np.ascontiguousarray(p[b, hf * half:(hf + 1) * half])
        cst, band = make_consts(cfg, hf == 0)
        m["cst"] = cst
        m["band"] = band
        in_maps.append(m)
    if "nc" not in _CACHE:
        _CACHE["nc"] = build(cfg)
    res = run_bass_kernel_spmd(_CACHE["nc"], in_maps, core_ids=list(range(ncores)))
    out = np.zeros((B, SEQ, D), np.float32)
    for cidx in range(ncores):
        b, hf = divmod(cidx, 2)
        out[b, hf * half:(hf + 1) * half] = res.results[cidx]["out"]
    return out
```
